# Optimizing a Trainium2 kernel written in Bass

```python
import jax, jax.numpy as jnp
from jax import lax
import numpy as np

D_MODEL = 2048
BATCH = 4
SEQ = 2048
DEPTH = 2

N_MIXERS = 2
N_SUBLAYERS = 3
D_FF = 5632
POOL_GROUPS = 4
POOL_WINDOWS = (2, 4, 8, 16)
POOL_GROUP_DIM = D_MODEL // POOL_GROUPS
N_HEADS = 16
N_KV_HEADS = 4
KV_GROUP = N_HEADS // N_KV_HEADS
HEAD_DIM = D_MODEL // N_HEADS
ROT_DIM = HEAD_DIM // 4
ROPE_THETA = 500000.0
IDX_HEADS = 16
IDX_DIM = 64
IDX_ROT_DIM = IDX_DIM // 4
TOPK_MAX = 256
Q_BLOCK = 128
EPS = 1e-6
NEG = -1e30
Q_W = N_HEADS * HEAD_DIM
KV_W = N_KV_HEADS * HEAD_DIM
QI_W = IDX_HEADS * IDX_DIM
DSA_IN = Q_W + 2 * KV_W + QI_W + IDX_DIM + IDX_HEADS
DSA_SPLITS = (Q_W, Q_W + KV_W, Q_W + 2 * KV_W, Q_W + 2 * KV_W + QI_W,
              Q_W + 2 * KV_W + QI_W + IDX_DIM)
N_POOL_LAYERS = (DEPTH + 1) // 2
N_DSA_LAYERS = DEPTH // 2

kernel_name = "hybrid_pool_dsa_macaron_adaln"


def rmsnorm(h, g):
    hf = h.astype(jnp.float32)
    hf = hf * lax.rsqrt(jnp.mean(hf * hf, axis=-1, keepdims=True) + EPS)
    return (hf * g.astype(jnp.float32)).astype(h.dtype)


def modulate(h, shift, scale):
    return h * (1 + scale[:, None, :]) + shift[:, None, :]


def partial_rope(x, positions, rot_dim):
    half = rot_dim // 2
    inv_freq = ROPE_THETA ** (-jnp.arange(half, dtype=jnp.float32) / half)
    ang = positions.astype(jnp.float32)[..., None] * inv_freq
    cos = jnp.cos(ang)[:, :, None, :]
    sin = jnp.sin(ang)[:, :, None, :]
    xr = x[..., :rot_dim].astype(jnp.float32)
    x1, x2 = xr[..., :half], xr[..., half:]
    rot = jnp.concatenate([x1 * cos - x2 * sin, x2 * cos + x1 * sin], axis=-1)
    return jnp.concatenate([rot.astype(x.dtype), x[..., rot_dim:]], axis=-1)


def swiglu_ffn(h, w_gu, w_d):
    g, u = jnp.split(h @ w_gu, 2, axis=-1)
    return (jax.nn.silu(g) * u) @ w_d


def pool_mixer(h, w_in, w_grp, ls, w_out):
    B, S, _ = h.shape
    u = (h @ w_in).reshape(B, S, POOL_GROUPS, POOL_GROUP_DIM)
    cs = jnp.cumsum(u.astype(jnp.float32), axis=1)
    t = jnp.arange(S)
    outs = []
    for g, w in enumerate(POOL_WINDOWS):
        c_g = cs[:, :, g]
        c_lag = jnp.pad(c_g, ((0, 0), (w, 0), (0, 0)))[:, :S]
        cnt = jnp.minimum(t + 1, w).astype(jnp.float32)[None, :, None]
        outs.append(((c_g - c_lag) / cnt - u[:, :, g].astype(jnp.float32)).astype(h.dtype))
    p = jnp.stack(outs, axis=2)
    v = jnp.einsum('bsgc,gcd->bsgd', p, w_grp).reshape(B, S, D_MODEL)
    return (v * ls) @ w_out


def dsa_mixer(h, positions, w_in, w_out):
    B, S, _ = h.shape
    q, k, v, qi, ki, wi = jnp.split(h @ w_in, DSA_SPLITS, axis=-1)
    q = partial_rope(q.reshape(B, S, N_HEADS, HEAD_DIM), positions, ROT_DIM)
    k = partial_rope(k.reshape(B, S, N_KV_HEADS, HEAD_DIM), positions, ROT_DIM)
    v = v.reshape(B, S, N_KV_HEADS, HEAD_DIM)
    qi = partial_rope(qi.reshape(B, S, IDX_HEADS, IDX_DIM), positions, IDX_ROT_DIM)
    ki = partial_rope(ki.reshape(B, S, 1, IDX_DIM), positions, IDX_ROT_DIM)[:, :, 0]
    ki32 = ki.astype(jnp.float32)
    top_k = min(TOPK_MAX, S // 4)
    nb = S // Q_BLOCK
    key_pos = jnp.arange(S)
    bidx = jnp.arange(B)[:, None, None]

    def to_blocks(a):
        return jnp.moveaxis(a.reshape(B, nb, Q_BLOCK, *a.shape[2:]), 1, 0)

    def block(args):
        blk, q_b, qi_b, wi_b = args
        t_pos = blk * Q_BLOCK + jnp.arange(Q_BLOCK)
        causal = key_pos[None, :] <= t_pos[:, None]
        rel = jax.nn.relu(jnp.einsum('bthd,bsd->bths', qi_b.astype(jnp.float32), ki32))
        score = jnp.einsum('bths,bth->bts', rel, wi_b.astype(jnp.float32))
        score = jnp.where(causal[None], score, NEG)
        _, sel = lax.top_k(score, top_k)
        valid = sel <= t_pos[None, :, None]
        k_sel = k[bidx, sel]
        v_sel = v[bidx, sel]
        qg = q_b.reshape(B, Q_BLOCK, N_KV_HEADS, KV_GROUP, HEAD_DIM)
        logits = jnp.einsum('btgrd,btkgd->btgrk', qg.astype(jnp.float32),
                            k_sel.astype(jnp.float32)) * (HEAD_DIM ** -0.5)
        logits = jnp.where(valid[:, :, None, None, :], logits, NEG)
        prob = jax.nn.softmax(logits, axis=-1)
        o = jnp.einsum('btgrk,btkgd->btgrd', prob.astype(v.dtype), v_sel)
        return o.reshape(B, Q_BLOCK, Q_W)

    out = lax.map(block, (jnp.arange(nb), to_blocks(q), to_blocks(qi), to_blocks(wi)))
    out = jnp.moveaxis(out, 0, 1).reshape(B, S, Q_W)
    return out @ w_out


def setup_inputs(seed: int = 0) -> dict:
    key = jax.random.key(seed)
    ks = jax.random.split(key, 16)
    f32 = jnp.float32
    nrm = lambda k, shape, s: jax.random.normal(k, shape, f32) * s
    x = nrm(ks[0], (BATCH, SEQ, D_MODEL), 1.0)
    c = nrm(ks[1], (BATCH, D_MODEL), 1.0)
    offsets = jax.random.randint(ks[2], (BATCH, 1), 0, 1024, dtype=jnp.int32)
    positions = offsets + jnp.arange(SEQ, dtype=jnp.int32)[None, :]
    ada_w = nrm(ks[3], (DEPTH, D_MODEL, N_SUBLAYERS * 3 * D_MODEL), 0.02)
    ada_b = nrm(ks[4], (DEPTH, N_SUBLAYERS * 3 * D_MODEL), 0.02)
    norm_g = 1.0 + nrm(ks[5], (DEPTH, N_SUBLAYERS, D_MODEL), 0.02)
    final_g = 1.0 + nrm(ks[6], (D_MODEL,), 0.02)
    ffn_wgu = nrm(ks[7], (DEPTH, 2, D_MODEL, 2 * D_FF), D_MODEL ** -0.5)
    ffn_wd = nrm(ks[8], (DEPTH, 2, D_FF, D_MODEL), D_FF ** -0.5)
    pool_w_in = nrm(ks[9], (N_POOL_LAYERS, D_MODEL, D_MODEL), D_MODEL ** -0.5)
    pool_w_grp = nrm(ks[10], (N_POOL_LAYERS, POOL_GROUPS, POOL_GROUP_DIM, POOL_GROUP_DIM), POOL_GROUP_DIM ** -0.5)
    pool_scale = 1.0 + nrm(ks[11], (N_POOL_LAYERS, D_MODEL), 0.1)
    pool_w_out = nrm(ks[12], (N_POOL_LAYERS, D_MODEL, D_MODEL), D_MODEL ** -0.5)
    dsa_w_in = nrm(ks[13], (N_DSA_LAYERS, D_MODEL, DSA_IN), D_MODEL ** -0.5)
    dsa_w_out = nrm(ks[14], (N_DSA_LAYERS, Q_W, D_MODEL), Q_W ** -0.5)
    return {"x": x, "c": c, "positions": positions, "ada_w": ada_w, "ada_b": ada_b,
            "norm_g": norm_g, "final_g": final_g, "ffn_wgu": ffn_wgu, "ffn_wd": ffn_wd,
            "pool_w_in": pool_w_in, "pool_w_grp": pool_w_grp, "pool_scale": pool_scale,
            "pool_w_out": pool_w_out, "dsa_w_in": dsa_w_in, "dsa_w_out": dsa_w_out}


def reference(x, c, positions, ada_w, ada_b, norm_g, final_g, ffn_wgu, ffn_wd,
              pool_w_in, pool_w_grp, pool_scale, pool_w_out, dsa_w_in, dsa_w_out):
    B = x.shape[0]
    ada = jnp.einsum('bd,lde->ble', jax.nn.silu(c), ada_w) + ada_b[None]
    ada = ada.reshape(B, DEPTH, N_SUBLAYERS, 3, D_MODEL)
    h = x
    for i in range(DEPTH):
        m = ada[:, i]
        y = modulate(rmsnorm(h, norm_g[i, 0]), m[:, 0, 0], m[:, 0, 1])
        h = h + 0.5 * m[:, 0, 2][:, None, :] * swiglu_ffn(y, ffn_wgu[i, 0], ffn_wd[i, 0])
        y = modulate(rmsnorm(h, norm_g[i, 1]), m[:, 1, 0], m[:, 1, 1])
        j = i // N_MIXERS
        if i % N_MIXERS == 0:
            y = pool_mixer(y, pool_w_in[j], pool_w_grp[j], pool_scale[j], pool_w_out[j])
        else:
            y = dsa_mixer(y, positions, dsa_w_in[j], dsa_w_out[j])
        h = h + m[:, 1, 2][:, None, :] * y
        y = modulate(rmsnorm(h, norm_g[i, 2]), m[:, 2, 0], m[:, 2, 1])
        h = h + 0.5 * m[:, 2, 2][:, None, :] * swiglu_ffn(y, ffn_wgu[i, 1], ffn_wd[i, 1])
    return rmsnorm(h, final_g)
```

```python
import math
import numpy as np
import concourse.bass as bass
import concourse.mybir as mybir
from concourse.bass_utils import run_bass_kernel_spmd

F32 = mybir.dt.float32
BF16 = mybir.dt.bfloat16
I32 = mybir.dt.int32
ALU = mybir.AluOpType
AF = mybir.ActivationFunctionType
AX = mybir.AxisListType

P = 128
D = 2048
KC = 16
T = 1024
TH = 16
TT = T + TH
DFF = 5632
S = 2048
NEG = -1.0e30
NEGB = -30000.0
EPS = 1e-6
THETA = 500000.0
MAIN = [(0, 512), (512, 512)]
WITH_HALO = MAIN + [(T, TH)]
FPARTS = [(0, 12), (12, 12), (24, 10), (34, 10)]
SLOT = 4096
NSLOT = 5
ESZ = {F32: 4, BF16: 2, I32: 4}


class View:
    def __init__(self, ap, arena, ranges):
        self.ap = ap
        self.arena = arena
        self.ranges = ranges


class Arena:
    def __init__(self, name, tensor, nbytes):
        self.name = name
        self.t = tensor
        self.nbytes = nbytes

    def view(self, off, shape, dt):
        e = ESZ[dt]
        n = int(np.prod(shape))
        assert off % 4 == 0 and off + n * e <= self.nbytes, (self.name, off, shape)
        ap = self.t[:, off // 2:(off + n * e) // 2]
        if dt != BF16:
            ap = ap.bitcast(dt)
        if len(shape) == 2:
            ap = ap.rearrange("p (a b) -> p a b", a=shape[0])
        elif len(shape) == 3:
            ap = ap.rearrange("p (a b c) -> p a b c", a=shape[0], b=shape[1])
        return Tile(self, off, tuple(shape), dt, ap)


class Tile:
    def __init__(self, arena, off, shape, dt, ap):
        self.arena, self.off, self.shape, self.dt, self.ap = arena, off, shape, dt, ap

    def _norm(self, idx):
        if not isinstance(idx, tuple):
            idx = (idx,)
        idx = list(idx) + [slice(None)] * (len(self.shape) - len(idx))
        out = []
        for i, s in zip(idx, self.shape):
            if isinstance(i, int):
                out.append((i, i + 1, True))
            else:
                a = 0 if i.start is None else i.start
                b = s if i.stop is None else i.stop
                out.append((a, b, False))
        return out

    def v(self, idx=(), p=None):
        nidx = self._norm(idx)
        e = ESZ[self.dt]
        strides = []
        acc = 1
        for s in reversed(self.shape):
            strides.append(acc)
            acc *= s
        strides = list(reversed(strides))
        ranges = []

        def rec(d, base):
            if d == len(nidx) - 1:
                a, b, _ = nidx[d]
                ranges.append((self.off + (base + a) * e, self.off + (base + b) * e))
                return
            a, b, _ = nidx[d]
            for i in range(a, b):
                rec(d + 1, base + i * strides[d])
        rec(0, 0)
        key = tuple(slice(a, b) if not isint else a for (a, b, isint) in nidx)
        pk = slice(None) if p is None else slice(p[0], p[1])
        ap = self.ap[(pk,) + key]
        return View(ap, self.arena, ranges)


class PS:
    def __init__(self, ap, bank):
        self.ap = ap
        self.arena = "psum"
        self.ranges = [(bank, bank + 1)]


BLK = 512


class Sched:
    COMPUTE = ("pe", "act", "dve", "pool")

    def __init__(self):
        self.ops = []
        self.q = {e: [] for e in ("pe", "act", "dve", "pool", "sp")}
        self.lastw = {}
        self.readers = {}
        self.known = {e: {} for e in self.q}

    def _blocks(self, views):
        out = set()
        for v in views:
            if v is None:
                continue
            name = v.arena if isinstance(v.arena, str) else v.arena.name
            for lo, hi in v.ranges:
                if name == "psum":
                    out.add((name, lo))
                else:
                    for b in range(lo // BLK, (hi - 1) // BLK + 1):
                        out.add((name, b))
        return out

    def add(self, eng, fn, reads=(), writes=(), kind="c"):
        oid = len(self.ops)
        rb = self._blocks(reads)
        wb = self._blocks(writes)
        deps = set()
        for b in rb:
            if b in self.lastw:
                deps.add(self.lastw[b])
        for b in wb:
            if b in self.lastw:
                deps.add(self.lastw[b])
            for r in self.readers.get(b, ()):
                deps.add(r)
        deps.discard(oid)
        op = dict(id=oid, eng=eng, fn=fn, kind=kind, deps=deps, signal=False, idx=len(self.q[eng]))
        for b in rb:
            self.readers.setdefault(b, []).append(oid)
        for b in wb:
            self.lastw[b] = oid
            self.readers[b] = []
        self.ops.append(op)
        self.q[eng].append(op)
        return oid

    def finalize(self, nring=12):
        for op in self.ops:
            eng = op["eng"]
            need = []
            best = {}
            for d in op["deps"]:
                dop = self.ops[d]
                if dop["kind"] == "c":
                    if dop["eng"] == eng and eng in ("pe", "sp"):
                        continue
                    k = dop["eng"]
                    if k not in best or dop["idx"] > best[k]["idx"]:
                        best[k] = dop
                else:
                    need.append(dop)
            for k, dop in best.items():
                if self.known[eng].get(k, -1) >= dop["idx"]:
                    continue
                self.known[eng][k] = dop["idx"]
                need.append(dop)
            op["need"] = need
            for dop in need:
                dop["signal"] = True
        cnt = {e: 0 for e in self.COMPUTE}
        dcount = {e: 0 for e in self.q}
        ccount = 0
        for op in self.ops:
            if op["kind"] == "c":
                if op["signal"]:
                    cnt[op["eng"]] += 1
                    op["sem"] = ("c", op["eng"])
                    op["val"] = cnt[op["eng"]]
            elif op["kind"] == "d":
                e = op["eng"]
                n = dcount[e]
                dcount[e] += 1
                op["sem"] = ("d", e, n % nring)
                op["val"] = 16 * (n // nring + 1)
                op["prev"] = (("d", e, n % nring), 16 * (n // nring)) if n >= nring else None
            else:
                ccount += 1
                op["sem"] = ("cc",)
                op["val"] = ccount
        self.nring = nring

    def emit(self, nc, block, sems):
        engs = {"pe": block.tensor, "act": block.scalar, "dve": block.vector, "pool": block.gpsimd, "sp": block.sync}
        for ename, deco in engs.items():
            ops = self.q[ename]
            if not ops:
                continue

            def body(e, ops=ops):
                for op in ops:
                    waits = []
                    for dop in op["need"]:
                        waits.append((dop["sem"], dop["val"]))
                    if op["kind"] == "d" and op.get("prev") is not None:
                        waits.append(op["prev"])
                    for sk, val in waits:
                        e.wait_ge(sems[sk], val)
                    ins = op["fn"](e)
                    if op["kind"] == "d":
                        ins.then_inc(sems[op["sem"]], 16)
                    elif op["kind"] == "cc":
                        ins.then_inc(sems[op["sem"]])
                    elif op["signal"]:
                        ins.then_inc(sems[op["sem"]], 1)
            deco(body)


def build(stop="full"):
    nc = bass.Bass("TRN2", target_bir_lowering=False)
    sc = Sched()
    dram = {}

    def din(name, shape, dt=F32):
        if name not in dram:
            dram[name] = nc.dram_tensor(name, list(shape), dt, kind="ExternalInput").ap()
        return dram[name]

    outT = nc.dram_tensor("outT", [D, T], F32, kind="ExternalOutput").ap()

    NB_H = KC * TT * 4
    NB_Y = KC * TT * 2
    NB_W = 44 * 1024
    NB_T = 16 * 1024
    NB_M = 16 * 1024
    th = nc.alloc_sbuf_tensor("R_h", [P, NB_H // 2], BF16)
    ty = nc.alloc_sbuf_tensor("R_y", [P, NB_Y // 2], BF16)
    tb = nc.alloc_sbuf_tensor("R_b", [P, NB_Y // 2], BF16)
    tw = nc.alloc_sbuf_tensor("R_w", [P, NB_W // 2], BF16)
    ttm = nc.alloc_sbuf_tensor("R_t", [P, NB_T // 2], BF16)
    tm = nc.alloc_sbuf_tensor("R_m", [P, NB_M // 2], BF16)
    A_h = Arena("h", th, NB_H)
    A_y = Arena("y", ty, NB_Y)
    A_b = Arena("b", tb, NB_Y)
    A_w = Arena("w", tw, NB_W)
    A_t = Arena("t", ttm, NB_T)
    A_m = Arena("m", tm, NB_M)

    hT = A_h.view(0, (KC, TT), F32)
    yT = A_y.view(0, (KC, TT), BF16)
    bT = A_b.view(0, (KC, TT), BF16)

    moff = [0]

    def malloc(shape, dt):
        n = int(np.prod(shape)) * ESZ[dt]
        n = (n + 3) // 4 * 4
        t = A_m.view(moff[0], shape, dt)
        moff[0] += n
        assert moff[0] <= NB_M, moff[0]
        return t

    modT = malloc((18, KC), F32)
    gT = malloc((7, KC), F32)
    lsT = malloc((KC,), F32)
    cpk = malloc((16,), F32)
    invc = malloc((4, 16), F32)
    frq = malloc((2,), F32)
    ident4 = malloc((512,), BF16)
    onesb = malloc((128,), BF16)
    pswq = malloc((128,), F32)
    pswi = malloc((128,), F32)
    stg = malloc((2, 512), BF16)
    m8 = malloc((8,), F32)
    thr = malloc((2,), F32)
    rp = malloc((8,), F32)
    wiT = malloc((8, 16), F32)
    negb = malloc((512,), BF16)
    kmx = malloc((8,), F32)
    smallw = malloc((1024,), F32)

    tmpA = A_t.view(0, (4, 1024), F32)
    qiT = A_t.view(0, (8, T), BF16)

    pst = [nc.alloc_psum_tensor(f"ps{i}", [P, 512], F32) for i in range(8)]

    def ps(i, n=512, p=None, dt=None):
        ap = pst[i][:, 0:n] if p is None else pst[i][p[0]:p[1], 0:n]
        return PS(ap, i)

    wslot = [0]

    def wtile(shape):
        n = int(np.prod(shape))
        assert n <= SLOT
        s = wslot[0] % NSLOT
        wslot[0] += 1
        return A_w.view(s * SLOT * 2, shape, BF16)

    def dma(eng, out_v, in_ap, kind="d"):
        sc.add(eng, lambda e, o=out_v.ap, i=in_ap: e.dma_start(out=o, in_=i), reads=(), writes=(out_v,), kind=kind)

    def dma_out(eng, out_ap, in_v):
        sc.add(eng, lambda e, o=out_ap, i=in_v.ap: e.dma_start(out=o, in_=i), reads=(in_v,), writes=(), kind="d")

    def loadw(dram_view, shape):
        t = wtile(shape)
        v = t.v()
        dma("pool", v, dram_view)
        return t

    def mm(out, lhsT, rhs, start, stop):
        sc.add("pe", lambda e, o=out.ap, l=lhsT.ap, r=rhs.ap, s0=start, s1=stop: e.matmul(o, l, r, start=s0, stop=s1),
               reads=(lhsT, rhs), writes=(out,))

    def act(out, in_, func, bias=None, scale=None, reads=()):
        kw = {}
        if bias is not None:
            kw["bias"] = bias.ap if isinstance(bias, View) else bias
        if scale is not None:
            kw["scale"] = scale.ap if isinstance(scale, View) else scale
        rd = [in_] + [x for x in (bias, scale) if isinstance(x, View)] + list(reads)
        sc.add("act", lambda e, o=out.ap, i=in_.ap, f=func, kw=kw: e.activation(o, i, f, **kw), reads=rd, writes=(out,))

    def tt(out, a, b, op, eng="dve"):
        sc.add(eng, lambda e, o=out.ap, x=a.ap, y=b.ap, op=op: e.tensor_tensor(o, x, y, op), reads=(a, b), writes=(out,))

    def ts(out, a, s1, s2, op0, op1=None, eng="dve"):
        rd = [a] + [x for x in (s1, s2) if isinstance(x, View)]
        a1 = s1.ap if isinstance(s1, View) else s1
        a2 = s2.ap if isinstance(s2, View) else s2
        if op1 is None:
            sc.add(eng, lambda e, o=out.ap, x=a.ap: e.tensor_scalar(o, x, a1, None, op0), reads=rd, writes=(out,))
        else:
            sc.add(eng, lambda e, o=out.ap, x=a.ap: e.tensor_scalar(o, x, a1, a2, op0, op1), reads=rd, writes=(out,))

    def stt(out, a, s, b, op0, op1):
        rd = [a, b] + ([s] if isinstance(s, View) else [])
        a1 = s.ap if isinstance(s, View) else s
        sc.add("dve", lambda e, o=out.ap, x=a.ap, y=b.ap: e.scalar_tensor_tensor(o, x, a1, y, op0, op1), reads=rd, writes=(out,))

    def cp(out, a, eng="dve"):
        sc.add(eng, lambda e, o=out.ap, x=a.ap: e.tensor_copy(o, x), reads=(a,), writes=(out,))

    cst = din("cst", [P, 1024])
    cstage = tmpA.v((slice(0, 1),))
    dma("sp", cstage, cst.rearrange("p (a n) -> p a n", a=1))
    cs = tmpA

    def csl(a, b):
        return cs.v((0, slice(a, b)))
    cp(ident4.v(), csl(0, 512))
    cp(onesb.v(), csl(512, 640))
    cp(pswq.v(), csl(640, 768))
    cp(pswi.v(), csl(768, 896))
    cp(frq.v(), csl(896, 898))
    cp(cpk.v(), csl(898, 914))
    cp(invc.v(), A_t.view(914 * 4, (4, 16), F32).v())
    cp(lsT.v(), csl(978, 994))
    gin = din("gT", [P, 7 * KC])
    dma("sp", gT.v(), gin.rearrange("p (a n) -> p a n", a=7))

    xT = din("xT", [D, T])
    xh = din("xhT", [D, TH])
    xv = xT.rearrange("(c p) t -> p c t", p=P)
    for c0 in range(0, KC, 4):
        dma("sp", hT.v((slice(c0, c0 + 4), slice(0, T))), xv[:, c0:c0 + 4, :])
    dma("sp", hT.v((slice(0, KC), slice(T, TT))), xh.rearrange("(c p) t -> p c t", p=P))

    cTin = din("cT", [P, KC * 4])
    awin = din("aw", [18 * D, 256])
    abin = din("ab", [P, 36])
    scf = A_t.view(8192, (KC, 4), F32)
    scb = A_t.view(8192 + 256, (KC, 4), BF16)
    abt = A_t.view(8192 + 512, (36,), F32)
    adaP = A_t.view(8192 + 1024, (36, 4), F32)
    dma("sp", scf.v(), cTin.rearrange("p (k b) -> p k b", b=4))
    dma("sp", abt.v(), abin)
    act(scb.v(), scf.v(), AF.Silu)
    pada = PS(pst[7][:, 0:144].rearrange("p (a b) -> p a b", b=4), 7)
    for lv in range(18):
        wt = loadw(awin[lv * D:(lv + 1) * D, :].rearrange("(k p) n -> p k n", p=P), (KC, 256))
        for j in range(2):
            o = PS(pst[7][:, (lv * 2 + j) * 4:(lv * 2 + j) * 4 + 4], 7)
            for k in range(KC):
                mm(o, wt.v((k, slice(j * 128, (j + 1) * 128))), scb.v((k,)), k == 0, k == KC - 1)
    sc.add("dve", lambda e: e.tensor_tensor(adaP.ap, pada.ap, abt.ap.unsqueeze(2).to_broadcast([P, 36, 4]), ALU.add),
           reads=(pada, abt.v()), writes=(adaP.v(),))
    ada_in = nc.dram_tensor("ada_in", [P, 144], F32).ap()
    ada_out = nc.dram_tensor("ada_out", [8 * P, 144], F32).ap()
    dflag = View(None, "dramflag", [(0, 1)])
    sc.add("pool", lambda e: e.dma_start(out=ada_in, in_=adaP.ap.rearrange("p a b -> p (a b)")),
           reads=(adaP.v(),), writes=(dflag,), kind="d")
    sc.add("pool", lambda e: e.collective_compute("AllGather", ALU.bypass, replica_groups=[list(range(8))],
                                                  ins=[ada_in.opt()], outs=[ada_out.opt()]),
           reads=(dflag,), writes=(dflag,), kind="cc")
    adaG = A_t.view(0, (8, 36, 4), F32)
    sc.add("pool", lambda e: e.dma_start(out=adaG.ap, in_=ada_out.rearrange("(r p) (a b) -> p r a b", p=P, b=4)),
           reads=(dflag, ), writes=(adaG.v(),), kind="d")
    adaM = A_t.view(4608, (8, 36, 4), F32)
    adaS = A_t.view(9216, (8, 36), F32)
    sel = cpk.v((slice(4, 8),))
    sc.add("dve", lambda e: e.tensor_tensor(adaM.ap, adaG.ap, sel.ap.unsqueeze(1).unsqueeze(1).to_broadcast([P, 8, 36, 4]), ALU.mult),
           reads=(adaG.v(), sel), writes=(adaM.v(),))
    sc.add("dve", lambda e: e.tensor_reduce(adaS.ap, adaM.ap, AX.X, ALU.add), reads=(adaM.v(),), writes=(adaS.v(),))
    for l in range(2):
        for s in range(3):
            base = (l * 3 + s) * 3

            def adav(k, l=l, s=s):
                a0 = (l * 9 + s * 3 + k) * 2
                return View(adaS.ap[:, :, a0:a0 + 2], A_t, adaS.v().ranges)
            gv = View(gT.ap[:, l * 3 + s, :].rearrange("p (r j) -> p r j", j=2), A_m, gT.v((l * 3 + s,)).ranges)

            def mv(i):
                return View(modT.ap[:, base + i, :].rearrange("p (r j) -> p r j", j=2), A_m, modT.v((base + i,)).ranges)
            stt(mv(0), adav(1), 1.0, gv, ALU.add, ALU.mult)
            cp(mv(1), adav(0))
            ts(mv(2), adav(2), 0.5 if s != 1 else 1.0, None, ALU.mult)

    def modv(l, s, i, c):
        return modT.v(((l * 3 + s) * 3 + i, slice(c, c + 1)))

    rstd = smallw

    def norm_mod(l, s, tchunks, final=False):
        for (t0, n) in tchunks:
            pn = ps(6, n)
            for c in range(KC):
                sq = stg.v((c % 2, slice(0, n)))
                act(sq, hT.v((c, slice(t0, t0 + n))), AF.Square)
                mm(pn, onesb.v(), sq, c == 0, c == KC - 1)
            r0 = rstd.v((slice(0, n),))
            act(r0, pn, AF.Sqrt, bias=EPS_T.v(), scale=1.0 / D)
            sc.add("dve", lambda e, o=r0.ap: e.reciprocal(o, o), reads=(r0,), writes=(r0,))
            for c in range(KC):
                hv = hT.v((c, slice(t0, t0 + n)))
                if final:
                    tmp = tmpA.v((c % 2, slice(0, n)))
                    tt(tmp, hv, r0, ALU.mult)
                    act(hv, tmp, AF.Identity, scale=gT.v((6, slice(c, c + 1))))
                else:
                    tmp = tmpA.v((c % 2, slice(0, n)))
                    tt(tmp, hv, r0, ALU.mult)
                    act(yT.v((c, slice(t0, t0 + n))), tmp, AF.Identity, bias=modv(l, s, 1, c), scale=modv(l, s, 0, c))

    EPS_T = malloc((1,), F32)
    sc.add("dve", lambda e: e.memset(EPS_T.ap, EPS), writes=(EPS_T.v(),))

    def resid_add(l, s, c, t0, n, pso):
        hv = hT.v((c, slice(t0, t0 + n)))
        stt(hv, pso, modv(l, s, 2, c), hv, ALU.mult, ALU.add)

    def ffn(l, j, tchunks):
        s = 0 if j == 0 else 2
        wgu = din("ffn_wgu", [2, 2, D, 2 * DFF])[l, j].rearrange("(k p) n -> p k n", p=P)
        wd = din("ffn_wd", [2, 2, DFF, D])[l, j]
        pi = [0]
        for (f0, nf) in FPARTS:
            for pr in range(nf // 2):
                fa = f0 + pr * 2
                wg = loadw(wgu[:, :, fa * 128:fa * 128 + 256], (KC, 256))
                wu = loadw(wgu[:, :, DFF + fa * 128:DFF + fa * 128 + 256], (KC, 256))
                for fc in range(2):
                    fl = fa + fc - f0
                    for (t0, n) in tchunks:
                        b = pi[0] % 2
                        pi[0] += 1
                        pg = ps(b * 2, n)
                        pu = ps(b * 2 + 1, n)
                        for k in range(KC):
                            mm(pg, wg.v((k, slice(fc * 128, fc * 128 + 128))), yT.v((k, slice(t0, t0 + n))), k == 0, k == KC - 1)
                        for k in range(KC):
                            mm(pu, wu.v((k, slice(fc * 128, fc * 128 + 128))), yT.v((k, slice(t0, t0 + n))), k == 0, k == KC - 1)
                        sg = tmpA.v((2 + b, slice(0, n)))
                        act(sg, pg, AF.Silu)
                        tt(bT.v((fl, slice(t0, t0 + n))), sg, pu, ALU.mult)
            for dp in range(8):
                wdt = loadw(wd[f0 * 128:(f0 + nf) * 128, dp * 256:(dp + 1) * 256].rearrange("(k p) n -> p k n", p=P), (nf, 256))
                for dc in range(2):
                    c = dp * 2 + dc
                    for (t0, n) in tchunks:
                        b = pi[0] % 2
                        pi[0] += 1
                        po = ps(4 + b, n)
                        for k in range(nf):
                            mm(po, wdt.v((k, slice(dc * 128, dc * 128 + 128))), bT.v((k, slice(t0, t0 + n))), k == 0, k == nf - 1)
                        resid_add(l, s, c, t0, n, po)

    def proj_resid(wv2d, src, l, s, tchunks):
        wv = wv2d.rearrange("(k p) n -> p k n", p=P)
        pi = 0
        for dp in range(8):
            wt = loadw(wv[:, :, dp * 256:(dp + 1) * 256], (KC, 256))
            for dc in range(2):
                c = dp * 2 + dc
                for (t0, n) in tchunks:
                    po = ps(4 + pi % 2, n)
                    pi += 1
                    for k in range(KC):
                        mm(po, wt.v((k, slice(dc * 128, dc * 128 + 128))), src.v((k, slice(t0, t0 + n))), k == 0, k == KC - 1)
                    resid_add(l, s, c, t0, n, po)

    def pool_mixer2(l):
        w_in = din("pool_w_in", [1, D, D])[0].rearrange("(k p) n -> p k n", p=P)
        w_grp = din("pool_w_grp", [1, 4, 512, 512])[0]
        w_out = din("pool_w_out", [1, D, D])[0]
        u0 = A_t.view(0, (TT,), F32)
        sA = A_t.view(TT * 4, (TT,), F32)
        sB = A_t.view(2 * TT * 4, (TT,), F32)
        pT = bT
        flag = cpk.v((slice(0, 1),))
        for dp in range(8):
            wt = loadw(w_in[:, :, dp * 256:(dp + 1) * 256], (KC, 256))
            for dc in range(2):
                c = dp * 2 + dc
                g = c // 4
                w = 2 ** (g + 1)
                for ti, (t0, n) in enumerate(WITH_HALO):
                    po = ps(ti % 2, n)
                    for k in range(KC):
                        mm(po, wt.v((k, slice(dc * 128, dc * 128 + 128))), yT.v((k, slice(t0, t0 + n))), k == 0, k == KC - 1)
                    if t0 == T:
                        act(u0.v((slice(0, TH),)), po, AF.Identity, scale=flag)
                    else:
                        act(u0.v((slice(TH + t0, TH + t0 + n),)), po, AF.Identity)
                src, step = u0, 1
                dsts = [sA, sB]
                di = 0
                while step < w:
                    lo = 2 * step
                    dst = dsts[di]
                    di = 1 - di
                    tt(dst.v((slice(lo, TT),)), src.v((slice(lo, TT),)), src.v((slice(lo - step, TT - step),)), ALU.add)
                    src, step = dst, step * 2
                stt(pT.v((c, slice(0, T))), src.v((slice(TH, TT),)), 1.0 / w, u0.v((slice(TH, TT),)), ALU.mult, ALU.subtract)
                t16 = dsts[di].v((slice(0, 16),))
                tt(t16, src.v((slice(TH, TH + 16),)), invc.v((g,)), ALU.mult)
                tt(pT.v((c, slice(0, 16))), t16, u0.v((slice(TH, TH + 16),)), ALU.subtract)
        pi = 0
        for g in range(4):
            for hp in range(2):
                wt = loadw(w_grp[g, :, hp * 256:(hp + 1) * 256].rearrange("(k p) n -> p k n", p=P), (4, 256))
                for dc in range(2):
                    c = g * 4 + hp * 2 + dc
                    for (t0, n) in MAIN:
                        po = ps(pi % 2, n)
                        pi += 1
                        for k in range(4):
                            mm(po, wt.v((k, slice(dc * 128, dc * 128 + 128))), pT.v((g * 4 + k, slice(t0, t0 + n))), k == 0, k == 3)
                        act(yT.v((c, slice(t0, t0 + n))), po, AF.Identity, scale=lsT.v((slice(c, c + 1),)))
        proj_resid(w_out, yT, l, 1, MAIN)

    def dsa_mixer(l):
        w_in = din("dsa_w_in", [1, D, 4176])[0].rearrange("(k p) n -> p k n", p=P)
        w_out = din("dsa_w_out", [1, D, D])[0]
        pos = din("pos", [P, T], I32)
        qT = bT
        RT = A_w.view(3 * SLOT * 2, (4, T), F32)
        pi32 = A_t.view(0, (T,), I32)
        pf = A_t.view(4096, (T,), F32)
        ang = A_t.view(8192, (T,), F32)
        kk = A_t.view(12288, (T,), I32)
        sc.add("sp", lambda e: e.dma_start(out=pi32.ap, in_=pos), writes=(pi32.v(),), kind="d")
        cp(pf.v(), pi32.v())
        TWO_PI = 2.0 * math.pi
        for var in range(2):
            for cs_i, shift in enumerate((math.pi / 2, 0.0)):
                dstt = RT.v((var * 2 + cs_i,))
                ts(ang.v(), pf.v(), frq.v((slice(var, var + 1),)), shift, ALU.mult, ALU.add)
                ts(kk.v(), ang.v(), 1.0 / TWO_PI, None, ALU.mult)
                cp(dstt, kk.v())
                stt(ang.v(), dstt, -TWO_PI, ang.v(), ALU.mult, ALU.add)
                ts(ang.v(), ang.v(), 3.14159, -3.14159, ALU.min, ALU.max)
                act(dstt, ang.v(), AF.Sin)
        wslot[0] = 0
        NS_SAVE = 3

        def loadw4(dram_view, shape):
            n = int(np.prod(shape))
            s_ = wslot[0] % NS_SAVE
            wslot[0] += 1
            t = A_w.view(s_ * SLOT * 2, shape, BF16)
            dma("pool", t.v(), dram_view)
            return t

        kx_in = nc.dram_tensor("kx_in", [576, T], BF16).ap()
        kx_out = nc.dram_tensor("kx_out", [2 * 576, T], BF16).ap()
        vx_in = nc.dram_tensor("vx_in", [T, 512], BF16).ap()
        vx_out = nc.dram_tensor("vx_out", [2 * T, 512], BF16).ap()
        kxf = View(None, "kxflag", [(0, 1)])
        vxf = View(None, "vxflag", [(0, 1)])

        x32b = [A_t.view(0, (512,), F32), A_t.view(2048, (512,), F32)]
        ui = [0]

        def rope_chunk(pso, npart, var, t0, dst_view):
            i = ui[0] % 2
            ui[0] += 1
            x32 = View(XR[i].ap[0:npart, :], A_m, XR[i].v().ranges)
            cp(x32, pso, eng="act" if False else "dve")
            psw = pswq if var == 0 else pswi
            pw = PS(pst[6][0:npart, 0:512], 6)
            mm(pw, View(psw.ap[0:npart, 0:npart], A_m, psw.v().ranges), x32, True, True)
            cosv = View(RT.ap[0:npart, var * 2, t0:t0 + 512], A_w, RT.v((var * 2, slice(t0, t0 + 512))).ranges)
            sinv = View(RT.ap[0:npart, var * 2 + 1, t0:t0 + 512], A_w, RT.v((var * 2 + 1, slice(t0, t0 + 512))).ranges)
            t1 = View(XS[i].ap[0:npart, :], A_m, XS[i].v().ranges)
            tt(t1, pw, sinv, ALU.mult)
            tt(x32, x32, cosv, ALU.mult)
            tt(dst_view, x32, t1, ALU.add)

        XR0 = malloc((512,), F32)
        XR = [XR0, XR0]
        XS0 = smallw.arena.view(smallw.off, (512,), F32)
        XS = [XS0, XS0]

        pj = [0]

        def nextps():
            b = pj[0] % 2
            pj[0] += 1
            return b
        for hp in range(8):
            wt = loadw4(w_in[:, :, hp * 256:(hp + 1) * 256], (KC, 256))
            for hc in range(2):
                h = hp * 2 + hc
                for (t0, n) in MAIN:
                    po = ps(nextps(), n)
                    for k in range(KC):
                        mm(po, wt.v((k, slice(hc * 128, hc * 128 + 128))), yT.v((k, slice(t0, t0 + n))), k == 0, k == KC - 1)
                    rope_chunk(po, 128, 0, t0, qT.v((h, slice(t0, t0 + n))))
        for hp in range(2):
            wt = loadw4(w_in[:, :, 2048 + hp * 256:2048 + (hp + 1) * 256], (KC, 256))
            for hc in range(2):
                g = hp * 2 + hc
                for (t0, n) in MAIN:
                    po = ps(nextps(), n)
                    for k in range(KC):
                        mm(po, wt.v((k, slice(hc * 128, hc * 128 + 128))), yT.v((k, slice(t0, t0 + n))), k == 0, k == KC - 1)
                    st = stg.v((ui[0] % 2,))
                    rope_chunk(po, 128, 0, t0, st)
                    sc.add("sp", lambda e, o=kx_in[g * 128:(g + 1) * 128, t0:t0 + n], i=st.ap: e.dma_start(out=o, in_=i),
                           reads=(st,), writes=(kxf,), kind="d")
        for hp in range(2):
            wt = loadw4(w_in[:, :, 2560 + hp * 256:2560 + (hp + 1) * 256], (KC, 256))
            for tb in range(8):
                po = ps(nextps(), 256)
                for k in range(KC):
                    mm(po, yT.v((k, slice(tb * 128, tb * 128 + 128))), wt.v((k,)), k == 0, k == KC - 1)
                st = stg.v((tb % 2, slice(0, 256)))
                cp(st, po)
                sc.add("sp", lambda e, o=vx_in[tb * 128:(tb + 1) * 128, hp * 256:(hp + 1) * 256], i=st.ap: e.dma_start(out=o, in_=i),
                       reads=(st,), writes=(vxf,), kind="d")
        wt = loadw4(w_in[:, :, 4096:4176], (KC, 80))
        for (t0, n) in MAIN:
            po = ps(nextps(), n, p=(0, 64))
            for k in range(KC):
                mm(po, wt.v((k, slice(0, 64))), yT.v((k, slice(t0, t0 + n))), k == 0, k == KC - 1)
            stt_ = stg.v((ui[0] % 2,))
            st = View(stt_.ap[0:64, :], A_m, stt_.ranges)
            rope_chunk(po, 64, 1, t0, st)
            sc.add("sp", lambda e, o=kx_in[512:576, t0:t0 + n], i=st.ap: e.dma_start(out=o, in_=i),
                   reads=(st,), writes=(kxf,), kind="d")
        for tb in range(8):
            po = ps(nextps(), 16)
            for k in range(KC):
                mm(po, yT.v((k, slice(tb * 128, tb * 128 + 128))), wt.v((k, slice(64, 80))), k == 0, k == KC - 1)
            cp(wiT.v((tb,)), po)
        sc.add("pool", lambda e: e.collective_compute("AllGather", ALU.bypass, replica_groups=[[0, 1], [2, 3], [4, 5], [6, 7]],
                                                      ins=[kx_in.opt()], outs=[kx_out.opt()]),
               reads=(kxf,), writes=(kxf,), kind="cc")
        sc.add("pool", lambda e: e.collective_compute("AllGather", ALU.bypass, replica_groups=[[0, 1], [2, 3], [4, 5], [6, 7]],
                                                      ins=[vx_in.opt()], outs=[vx_out.opt()]),
               reads=(vxf,), writes=(vxf,), kind="cc")
        for hp in range(4):
            wt = loadw4(w_in[:, :, 3072 + hp * 256:3072 + (hp + 1) * 256], (KC, 256))
            for hc in range(2):
                c = hp * 2 + hc
                for (t0, n) in MAIN:
                    po = ps(nextps(), n)
                    for k in range(KC):
                        mm(po, wt.v((k, slice(hc * 128, hc * 128 + 128))), yT.v((k, slice(t0, t0 + n))), k == 0, k == KC - 1)
                    rope_chunk(po, 128, 1, t0, qiT.v((c, slice(t0, t0 + n))))
        kTf = A_w.view(0, (4, S), BF16)
        Vf = A_w.view(16384, (16, 512), BF16)
        ki2 = A_w.view(32768, (S,), BF16)
        mb = A_w.view(36864, (S,), BF16)
        ptb = [A_w.view(40960 + i * 1024, (512,), BF16) for i in range(3)]
        Rr = [A_y.view(28672 + i * 2048, (512,), F32) for i in range(2)]
        kxo = kx_out.rearrange("(r x) s -> x r s", r=2)
        for g in range(4):
            sc.add("sp", lambda e, o=kTf.ap[:, g, :].rearrange("p (r s) -> p r s", r=2), i=kxo[g * 128:(g + 1) * 128]: e.dma_start(out=o, in_=i),
                   reads=(kxf,), writes=(kTf.v((g,)),), kind="d")
        for hh in range(2):
            kv = View(ki2.ap[hh * 64:(hh + 1) * 64, :].rearrange("p (r s) -> p r s", r=2), A_w, ki2.v().ranges)
            sc.add("sp", lambda e, o=kv.ap, i=kxo[512:576]: e.dma_start(out=o, in_=i), reads=(kxf,), writes=(kv,), kind="d")
        sc.add("sp", lambda e: e.dma_start(out=Vf.ap, in_=vx_out.rearrange("(b p) f -> p b f", p=P)), reads=(vxf,), writes=(Vf.v(),), kind="d")
        acc = A_y.view(0, (S,), F32)
        Wk = A_y.view(8192, (S,), F32)
        iot = A_y.view(16384, (S,), F32)
        ksq = A_y.view(24576, (512,), BF16)
        sc.add("pool", lambda e: e.iota(iot.ap, [[1, S]], base=0, channel_multiplier=0, allow_small_or_imprecise_dtypes=True),
               reads=(), writes=(iot.v(),))
        for i in range(8):
            ts(rp.v((slice(i, i + 1),)), cpk.v((slice(1, 2),)), float(i * 128), None, ALU.add)
        for g in range(4):
            for sb4 in range(4):
                kq = ksq.v()
                act(kq, kTf.v((g, slice(sb4 * 512, sb4 * 512 + 512))), AF.Square)
                pk = PS(pst[6][0:1, 0:512], 6)
                mm(pk, View(onesb.ap[:, 0:1], A_m, onesb.v().ranges), kq, True, True)
                dstk = View(smallw.ap[0:1, g * 4 + sb4:g * 4 + sb4 + 1], A_m, smallw.v().ranges)
                sc.add("dve", lambda e, o=dstk.ap, i=pk.ap: e.tensor_reduce(o, i, AX.X, ALU.max), reads=(pk,), writes=(dstk,))
            src4 = View(smallw.ap[0:1, g * 4:g * 4 + 4], A_m, smallw.v().ranges)
            kd = View(kmx.ap[0:1, g:g + 1], A_m, kmx.v().ranges)
            sc.add("dve", lambda e, o=kd.ap, i=src4.ap: e.tensor_reduce(o, i, AX.X, ALU.max), reads=(src4,), writes=(kd,))
        km4 = View(kmx.ap[0:1, 0:4], A_m, kmx.v().ranges)
        km4s = View(kmx.ap[0:1, 4:8], A_m, kmx.v().ranges)
        act(km4s, km4, AF.Sqrt)
        ts(km4s, km4s, -1.03, None, ALU.mult)
        SCALE = 1.0 / math.sqrt(128.0)
        for i in range(8):
            nkb = 9 + i
            nk = nkb * 128
            ts(acc.v((slice(0, nk),)), iot.v((slice(0, nk),)), rp.v((slice(i, i + 1),)), NEG, ALU.is_gt, ALU.mult)
            ri = 0
            for h in range(16):
                c, pb = h // 2, (h % 2) * 64
                k0 = 0
                while k0 < nk:
                    n = min(512, nk - k0)
                    b = nextps()
                    px = ps(b, n)
                    lh = View(qiT.ap[pb:pb + 64, c, i * 128:(i + 1) * 128], A_t, qiT.v((c, slice(i * 128, (i + 1) * 128))).ranges)
                    rh = View(ki2.ap[pb:pb + 64, k0:k0 + n], A_w, ki2.v((slice(k0, k0 + n),)).ranges)
                    mm(px, lh, rh, True, True)
                    rr = Rr[ri % 2].v((slice(0, n),))
                    ri += 1
                    act(rr, px, AF.Relu)
                    av = acc.v((slice(k0, k0 + n),))
                    stt(av, rr, wiT.v((i, slice(h, h + 1))), av, ALU.mult, ALU.add)
                    k0 += n
            for r in range(32):
                srcv = acc.v((slice(0, nk),)) if r == 0 else Wk.v((slice(0, nk),))
                sc.add("dve", lambda e, o=m8.ap, s_=srcv.ap: e.max(out=o, in_=s_), reads=(srcv,), writes=(m8.v(),))
                if r < 31:
                    wv = Wk.v((slice(0, nk),))
                    sc.add("dve", lambda e, o=wv.ap, s_=srcv.ap, m=m8.ap: e.match_replace(out=o, in_to_replace=m, in_values=s_, imm_value=-3.0e38),
                           reads=(srcv, m8.v()), writes=(wv,))
            ts(thr.v((slice(0, 1),)), m8.v((slice(7, 8),)), -1.0e29, None, ALU.max)
            ts(mb.v((slice(0, nk),)), acc.v((slice(0, nk),)), thr.v((slice(0, 1),)), NEGB, ALU.is_lt, ALU.mult)
            for g in range(4):
                qblk = View(qT.ap[:, 4 * g:4 * g + 4, i * 128:(i + 1) * 128], A_b,
                            qT.v((slice(4 * g, 4 * g + 4), slice(i * 128, (i + 1) * 128))).ranges)
                sq4 = View(ksq.ap.rearrange("p (a b) -> p a b", a=4), A_y, ksq.v().ranges)
                act(sq4, qblk, AF.Square)
                pq = PS(pst[6][0:1, 0:512], 6)
                mm(pq, View(onesb.ap[:, 0:1], A_m, onesb.v().ranges), ksq.v(), True, True)
                nb0 = View(smallw.ap[0:1, 512:1024], A_m, smallw.v().ranges)
                act(nb0, pq, AF.Sqrt)
                nbv = View(negb.ap[0:1, :], A_m, negb.v().ranges)
                ts(nbv, nb0, View(kmx.ap[0:1, 4 + g:5 + g], A_m, kmx.v().ranges), None, ALU.mult)
                po = ps(4)
                pd = ps(5)
                for sb in range(nkb):
                    pS = ps(2 + sb % 2)
                    mm(pS, kTf.v((g, slice(sb * 128, sb * 128 + 128))), qblk, True, False)
                    mm(pS, mb.v((slice(sb * 128, sb * 128 + 128),)), ident4.v(), False, False)
                    mm(pS, View(onesb.ap[0:1, :], A_m, onesb.v().ranges), nbv, False, True)
                    pt = ptb[sb % 3].v()
                    act(pt, pS, AF.Exp, scale=SCALE)
                    mm(po, Vf.v((sb, slice(g * 128, g * 128 + 128))), pt, sb == 0, sb == nkb - 1)
                    mm(pd, onesb.v(), pt, sb == 0, sb == nkb - 1)
                rd = tmpR.v()
                sc.add("dve", lambda e, o=rd.ap, i_=pd.ap: e.reciprocal(o, i_), reads=(pd,), writes=(rd,))
                ov = View(qblk.ap, A_b, qblk.ranges)
                sc.add("dve", lambda e, o=ov.ap, a=po.ap.rearrange("p (a b) -> p a b", a=4), b_=rd.ap.rearrange("p (a b) -> p a b", a=4):
                       e.tensor_tensor(o, a, b_, ALU.mult), reads=(po, rd), writes=(ov,))
        wslot[0] = 0
        proj_resid(w_out, qT, l, 1, MAIN)

    tmpR = A_y.view(26624, (512,), F32)

    stages = ["ffn1_0", "pool", "ffn2_0", "ffn1_1", "dsa", "full"]
    if stop == "pool_only":
        norm_mod(0, 1, WITH_HALO)
        pool_mixer2(0)
    elif stop == "dsa_only":
        norm_mod(1, 1, MAIN)
        dsa_mixer(1)
    else:
        si = stages.index(stop)
        norm_mod(0, 0, WITH_HALO)
        ffn(0, 0, WITH_HALO)
        if si >= 1:
            norm_mod(0, 1, WITH_HALO)
            pool_mixer2(0)
        if si >= 2:
            norm_mod(0, 2, MAIN)
            ffn(0, 1, MAIN)
        if si >= 3:
            norm_mod(1, 0, MAIN)
            ffn(1, 0, MAIN)
        if si >= 4:
            norm_mod(1, 1, MAIN)
            dsa_mixer(1)
        if si >= 5:
            norm_mod(1, 2, MAIN)
            ffn(1, 1, MAIN)
            norm_mod(0, 0, MAIN, final=True)
    ov = outT.rearrange("(c p) t -> p c t", p=P)
    for c0 in range(0, KC, 4):
        dma_out("sp", ov[:, c0:c0 + 4, :], hT.v((slice(c0, c0 + 4), slice(0, T))))
    last = [op for op in sc.q["sp"] if op["kind"] == "d"][-4:]
    sc.finalize()
    sems = {}
    for e in Sched.COMPUTE:
        sems[("c", e)] = nc.alloc_semaphore(f"c_{e}")
    for e in ("sp", "pool"):
        for i in range(sc.nring):
            sems[("d", e, i)] = nc.alloc_semaphore(f"d_{e}_{i}")
    sems[("cc",)] = nc.alloc_semaphore("ccs")
    with nc.Block() as block:
        def final_wait(e):
            for op in last:
                e.wait_ge(sems[op["sem"]], op["val"])
            return e.nop()
        fop = dict(id=-1, eng="sp", fn=final_wait, kind="c", deps=set(), need=[], signal=False, idx=len(sc.q["sp"]))
        sc.q["sp"].append(fop)
        sc.emit(nc, block, sems)
    return nc, list(dram.keys())


def _consts(hf):
    cst = np.zeros((P, 1024), np.float32)
    eye = np.eye(P, dtype=np.float32)
    cst[:, 0:512] = np.tile(eye, (1, 4))
    cst[:, 512:640] = 1.0
    def psw(block, half):
        m = np.zeros((P, P), np.float32)
        for b0 in range(0, P, block):
            for dd in range(half):
                m[b0 + dd + half, b0 + dd] = -1.0
                m[b0 + dd, b0 + dd + half] = 1.0
        return m
    cst[:, 640:768] = psw(128, 16)
    cst[:, 768:896] = psw(64, 8)
    fq = np.zeros(P, np.float32)
    fq[0:16] = THETA ** (-np.arange(16, dtype=np.float32) / 16)
    fq[16:32] = fq[0:16]
    fi = np.zeros(P, np.float32)
    for b0 in (0, 64):
        fi[b0:b0 + 8] = THETA ** (-np.arange(8, dtype=np.float32) / 8)
        fi[b0 + 8:b0 + 16] = fi[b0:b0 + 8]
    cst[:, 896] = fq
    cst[:, 897] = fi
    return cst


def kernel(x, c, positions, ada_w, ada_b, norm_g, final_g, ffn_wgu, ffn_wd,
           pool_w_in, pool_w_grp, pool_scale, pool_w_out, dsa_w_in, dsa_w_out, _stop="full"):
    nc, names = build(_stop)
    x = np.asarray(x, np.float32)
    f = lambda a: np.ascontiguousarray(np.asarray(a))
    gT = np.concatenate([np.asarray(norm_g, np.float32).reshape(6, D), np.asarray(final_g, np.float32).reshape(1, D)], 0)
    gT = f(gT.reshape(7, KC, P).transpose(2, 0, 1).reshape(P, 7 * KC))
    cT = f(np.asarray(c, np.float32).reshape(4, KC, P).transpose(2, 1, 0).reshape(P, KC * 4))
    aw = np.asarray(ada_w, np.float32).reshape(2, D, 9, D)
    ab = np.asarray(ada_b, np.float32).reshape(2, 9, D)
    ls = np.asarray(pool_scale, np.float32).reshape(KC, P).T
    in_maps = []
    for core in range(8):
        b, hf = core // 2, core % 2
        m = {}
        m["xT"] = f(x[b, hf * T:(hf + 1) * T, :].T)
        m["xhT"] = f(x[b, T - TH:T, :].T) if hf == 1 else np.zeros((D, TH), np.float32)
        cst = _consts(hf)
        cst[:, 898] = float(hf)
        cst[:, 899] = np.arange(P, dtype=np.float32) + hf * T
        cst[:, 902 + b] = 1.0
        for g, w in enumerate((2, 4, 8, 16)):
            tpos = np.arange(16) + hf * T
            cst[:, 914 + g * 16:914 + (g + 1) * 16] = (1.0 / np.minimum(tpos + 1, w)).astype(np.float32)[None, :]
        cst[:, 978:994] = ls
        m["cst"] = cst
        m["gT"] = gT
        m["cT"] = cT
        m["aw"] = f(aw[:, :, :, core * 256:(core + 1) * 256].transpose(0, 2, 1, 3).reshape(18 * D, 256))
        m["ab"] = f(ab[:, :, core * 256:(core + 1) * 256].reshape(18, 2, P).transpose(2, 0, 1).reshape(P, 36))
        m["pos"] = f(np.tile(np.asarray(positions, np.int32)[b, hf * T:(hf + 1) * T].reshape(1, T), (P, 1)))
        m["ffn_wgu"] = f(ffn_wgu)
        m["ffn_wd"] = f(ffn_wd)
        m["pool_w_in"] = f(pool_w_in)
        m["pool_w_grp"] = f(pool_w_grp)
        m["pool_w_out"] = f(pool_w_out)
        m["dsa_w_in"] = f(dsa_w_in)
        m["dsa_w_out"] = f(dsa_w_out)
        in_maps.append({k: m[k] for k in names})
    res = run_bass_kernel_spmd(nc, in_maps, core_ids=list(range(8)))
    out = np.zeros((4, S, D), np.float32)
    for core in range(8):
        b, hf = core // 2, core % 2
        out[b, hf * T:(hf + 1) * T, :] = res.results[core]["outT"].T
    return out
```

```python
import math
import numpy as np
import concourse.bass as bass
import concourse.mybir as mybir
from concourse.bass_utils import run_bass_kernel_spmd

F32 = mybir.dt.float32
BF16 = mybir.dt.bfloat16
I32 = mybir.dt.int32
I16 = mybir.dt.int16
ALU = mybir.AluOpType
AF = mybir.ActivationFunctionType
AX = mybir.AxisListType

P = 128
D = 2048
KC = 16
T = 1024
TH = 16
TT = T + TH
DFF = 5632
S = 2048
NEG = -1.0e30
NEGB = -30000.0
EPS = 1e-6
THETA = 500000.0
MAIN = [(0, 512), (512, 512)]
WITH_HALO = MAIN + [(T, TH)]
FPARTS = [(0, 12), (12, 12), (24, 10), (34, 10)]
SLOT = 4096
NSLOT = 5
ESZ = {F32: 4, BF16: 2, I32: 4, I16: 2}


class View:
    def __init__(self, ap, arena, ranges):
        self.ap = ap
        self.arena = arena
        self.ranges = ranges


class Arena:
    def __init__(self, name, tensor, nbytes):
        self.name = name
        self.t = tensor
        self.nbytes = nbytes

    def view(self, off, shape, dt):
        e = ESZ[dt]
        n = int(np.prod(shape))
        assert off % 4 == 0 and off + n * e <= self.nbytes, (self.name, off, shape)
        ap = self.t[:, off // 2:(off + n * e) // 2]
        if dt != BF16:
            ap = ap.bitcast(dt)
        if len(shape) == 2:
            ap = ap.rearrange("p (a b) -> p a b", a=shape[0])
        elif len(shape) == 3:
            ap = ap.rearrange("p (a b c) -> p a b c", a=shape[0], b=shape[1])
        return Tile(self, off, tuple(shape), dt, ap)


class Tile:
    def __init__(self, arena, off, shape, dt, ap):
        self.arena, self.off, self.shape, self.dt, self.ap = arena, off, shape, dt, ap

    def _norm(self, idx):
        if not isinstance(idx, tuple):
            idx = (idx,)
        idx = list(idx) + [slice(None)] * (len(self.shape) - len(idx))
        out = []
        for i, s in zip(idx, self.shape):
            if isinstance(i, int):
                out.append((i, i + 1, True))
            else:
                a = 0 if i.start is None else i.start
                b = s if i.stop is None else i.stop
                out.append((a, b, False))
        return out

    def v(self, idx=(), p=None):
        nidx = self._norm(idx)
        e = ESZ[self.dt]
        strides = []
        acc = 1
        for s in reversed(self.shape):
            strides.append(acc)
            acc *= s
        strides = list(reversed(strides))
        ranges = []

        def rec(d, base):
            if d == len(nidx) - 1:
                a, b, _ = nidx[d]
                ranges.append((self.off + (base + a) * e, self.off + (base + b) * e))
                return
            a, b, _ = nidx[d]
            for i in range(a, b):
                rec(d + 1, base + i * strides[d])
        rec(0, 0)
        key = tuple(slice(a, b) if not isint else a for (a, b, isint) in nidx)
        pk = slice(None) if p is None else slice(p[0], p[1])
        ap = self.ap[(pk,) + key]
        return View(ap, self.arena, ranges)


class PS:
    def __init__(self, ap, bank):
        self.ap = ap
        self.arena = "psum"
        self.ranges = [(bank, bank + 1)]


BLK = 512


class Sched:
    COMPUTE = ("pe", "act", "dve", "pool")

    def __init__(self):
        self.ops = []
        self.q = {e: [] for e in ("pe", "act", "dve", "pool", "sp")}
        self.lastw = {}
        self.readers = {}
        self.known = {e: {} for e in self.q}

    def _blocks(self, views):
        out = set()
        for v in views:
            if v is None:
                continue
            name = v.arena if isinstance(v.arena, str) else v.arena.name
            for lo, hi in v.ranges:
                if name == "psum":
                    out.add((name, lo))
                else:
                    for b in range(lo // BLK, (hi - 1) // BLK + 1):
                        out.add((name, b))
        return out

    def add(self, eng, fn, reads=(), writes=(), kind="c"):
        oid = len(self.ops)
        rb = self._blocks(reads)
        wb = self._blocks(writes)
        deps = set()
        for b in rb:
            if b in self.lastw:
                deps.add(self.lastw[b])
        for b in wb:
            if b in self.lastw:
                deps.add(self.lastw[b])
            for r in self.readers.get(b, ()):
                deps.add(r)
        deps.discard(oid)
        op = dict(id=oid, eng=eng, fn=fn, kind=kind, deps=deps, signal=False, idx=len(self.q[eng]))
        for b in rb:
            self.readers.setdefault(b, []).append(oid)
        for b in wb:
            self.lastw[b] = oid
            self.readers[b] = []
        self.ops.append(op)
        self.q[eng].append(op)
        return oid

    def finalize(self, nring=12):
        for op in self.ops:
            eng = op["eng"]
            need = []
            best = {}
            for d in op["deps"]:
                dop = self.ops[d]
                if dop["kind"] == "c":
                    if dop["eng"] == eng and eng in ("pe", "sp"):
                        continue
                    k = dop["eng"]
                    if k not in best or dop["idx"] > best[k]["idx"]:
                        best[k] = dop
                else:
                    need.append(dop)
            for k, dop in best.items():
                if self.known[eng].get(k, -1) >= dop["idx"]:
                    continue
                self.known[eng][k] = dop["idx"]
                need.append(dop)
            op["need"] = need
            for dop in need:
                dop["signal"] = True
        cnt = {e: 0 for e in self.COMPUTE}
        dcount = {e: 0 for e in self.q}
        ccount = 0
        for op in self.ops:
            if op["kind"] == "c":
                if op["signal"]:
                    cnt[op["eng"]] += 1
                    op["sem"] = ("c", op["eng"])
                    op["val"] = cnt[op["eng"]]
            elif op["kind"] == "d":
                e = op["eng"]
                n = dcount[e]
                dcount[e] += 1
                op["sem"] = ("d", e, n % nring)
                op["val"] = 16 * (n // nring + 1)
                op["prev"] = (("d", e, n % nring), 16 * (n // nring)) if n >= nring else None
            else:
                ccount += 1
                op["sem"] = ("cc",)
                op["val"] = ccount
        self.nring = nring

    def emit(self, nc, block, sems):
        engs = {"pe": block.tensor, "act": block.scalar, "dve": block.vector, "pool": block.gpsimd, "sp": block.sync}
        for ename, deco in engs.items():
            ops = self.q[ename]
            if not ops:
                continue

            def body(e, ops=ops):
                for op in ops:
                    waits = []
                    for dop in op["need"]:
                        waits.append((dop["sem"], dop["val"]))
                    if op["kind"] == "d" and op.get("prev") is not None:
                        waits.append(op["prev"])
                    for sk, val in waits:
                        e.wait_ge(sems[sk], val)
                    ins = op["fn"](e)
                    if op["kind"] == "d":
                        ins.then_inc(sems[op["sem"]], 16)
                    elif op["kind"] == "cc":
                        ins.then_inc(sems[op["sem"]])
                    elif op["signal"]:
                        ins.then_inc(sems[op["sem"]], 1)
            deco(body)


def build(stop="full"):
    nc = bass.Bass("TRN2", target_bir_lowering=False)
    sc = Sched()
    dram = {}

    def din(name, shape, dt=F32):
        if name not in dram:
            dram[name] = nc.dram_tensor(name, list(shape), dt, kind="ExternalInput").ap()
        return dram[name]

    outT = nc.dram_tensor("outT", [D, T], F32, kind="ExternalOutput").ap()

    NB_H = KC * TT * 4
    NB_Y = KC * TT * 2
    NB_W = 44 * 1024
    NB_T = 16 * 1024
    NB_M = 16 * 1024
    th = nc.alloc_sbuf_tensor("R_h", [P, NB_H // 2], BF16)
    ty = nc.alloc_sbuf_tensor("R_y", [P, NB_Y // 2], BF16)
    tb = nc.alloc_sbuf_tensor("R_b", [P, NB_Y // 2], BF16)
    tw = nc.alloc_sbuf_tensor("R_w", [P, NB_W // 2], BF16)
    ttm = nc.alloc_sbuf_tensor("R_t", [P, NB_T // 2], BF16)
    tm = nc.alloc_sbuf_tensor("R_m", [P, NB_M // 2], BF16)
    A_h = Arena("h", th, NB_H)
    A_y = Arena("y", ty, NB_Y)
    A_b = Arena("b", tb, NB_Y)
    A_w = Arena("w", tw, NB_W)
    A_t = Arena("t", ttm, NB_T)
    A_m = Arena("m", tm, NB_M)

    hT = A_h.view(0, (KC, TT), F32)
    yT = A_y.view(0, (KC, TT), BF16)
    bT = A_b.view(0, (KC, TT), BF16)

    moff = [0]

    def malloc(shape, dt, align=4):
        moff[0] = (moff[0] + align - 1) // align * align
        n = int(np.prod(shape)) * ESZ[dt]
        n = (n + 3) // 4 * 4
        t = A_m.view(moff[0], shape, dt)
        moff[0] += n
        assert moff[0] <= NB_M, moff[0]
        return t

    modT = malloc((18, KC), F32)
    gT = malloc((7, KC), F32)
    lsT = malloc((KC,), F32)
    cpk = malloc((16,), F32)
    invc = malloc((4, 16), F32)
    frq = malloc((2,), F32)
    ident4 = malloc((512,), BF16)
    onesb = malloc((128,), BF16)
    pswq = malloc((128,), F32)
    pswi = malloc((128,), F32)
    stg = malloc((2, 512), BF16)
    m8 = malloc((8,), F32)
    thr = malloc((2,), F32)
    rp = malloc((8,), F32)
    wiT = malloc((8, 16), F32)
    negb = malloc((512,), BF16)
    kmx = malloc((8,), F32)
    smallw = malloc((1024,), F32)

    tmpA = A_t.view(0, (4, 1024), F32)
    qiT = A_t.view(0, (8, T), BF16)

    pst = [nc.alloc_psum_tensor(f"ps{i}", [P, 512], F32) for i in range(8)]

    def ps(i, n=512, p=None, dt=None):
        ap = pst[i][:, 0:n] if p is None else pst[i][p[0]:p[1], 0:n]
        return PS(ap, i)

    wslot = [0]

    def wtile(shape):
        n = int(np.prod(shape))
        assert n <= SLOT
        s = wslot[0] % NSLOT
        wslot[0] += 1
        return A_w.view(s * SLOT * 2, shape, BF16)

    def dma(eng, out_v, in_ap, kind="d"):
        sc.add(eng, lambda e, o=out_v.ap, i=in_ap: e.dma_start(out=o, in_=i), reads=(), writes=(out_v,), kind=kind)

    def dma_out(eng, out_ap, in_v):
        sc.add(eng, lambda e, o=out_ap, i=in_v.ap: e.dma_start(out=o, in_=i), reads=(in_v,), writes=(), kind="d")

    def loadw(dram_view, shape):
        t = wtile(shape)
        v = t.v()
        dma("pool", v, dram_view)
        return t

    def mm(out, lhsT, rhs, start, stop):
        sc.add("pe", lambda e, o=out.ap, l=lhsT.ap, r=rhs.ap, s0=start, s1=stop: e.matmul(o, l, r, start=s0, stop=s1),
               reads=(lhsT, rhs), writes=(out,))

    def act(out, in_, func, bias=None, scale=None, reads=()):
        kw = {}
        if bias is not None:
            kw["bias"] = bias.ap if isinstance(bias, View) else bias
        if scale is not None:
            kw["scale"] = scale.ap if isinstance(scale, View) else scale
        rd = [in_] + [x for x in (bias, scale) if isinstance(x, View)] + list(reads)
        sc.add("act", lambda e, o=out.ap, i=in_.ap, f=func, kw=kw: e.activation(o, i, f, **kw), reads=rd, writes=(out,))

    def tt(out, a, b, op, eng="dve"):
        sc.add(eng, lambda e, o=out.ap, x=a.ap, y=b.ap, op=op: e.tensor_tensor(o, x, y, op), reads=(a, b), writes=(out,))

    def ts(out, a, s1, s2, op0, op1=None, eng="dve"):
        rd = [a] + [x for x in (s1, s2) if isinstance(x, View)]
        a1 = s1.ap if isinstance(s1, View) else s1
        a2 = s2.ap if isinstance(s2, View) else s2
        if op1 is None:
            sc.add(eng, lambda e, o=out.ap, x=a.ap: e.tensor_scalar(o, x, a1, None, op0), reads=rd, writes=(out,))
        else:
            sc.add(eng, lambda e, o=out.ap, x=a.ap: e.tensor_scalar(o, x, a1, a2, op0, op1), reads=rd, writes=(out,))

    def stt(out, a, s, b, op0, op1):
        rd = [a, b] + ([s] if isinstance(s, View) else [])
        a1 = s.ap if isinstance(s, View) else s
        sc.add("dve", lambda e, o=out.ap, x=a.ap, y=b.ap: e.scalar_tensor_tensor(o, x, a1, y, op0, op1), reads=rd, writes=(out,))

    def cp(out, a, eng="dve"):
        sc.add(eng, lambda e, o=out.ap, x=a.ap: e.tensor_copy(o, x), reads=(a,), writes=(out,))

    cst = din("cst", [P, 1024])
    cstage = tmpA.v((slice(0, 1),))
    dma("sp", cstage, cst.rearrange("p (a n) -> p a n", a=1))
    cs = tmpA

    def csl(a, b):
        return cs.v((0, slice(a, b)))
    cp(ident4.v(), csl(0, 512))
    cp(onesb.v(), csl(512, 640))
    cp(pswq.v(), csl(640, 768))
    cp(pswi.v(), csl(768, 896))
    cp(frq.v(), csl(896, 898))
    cp(cpk.v(), csl(898, 914))
    cp(invc.v(), A_t.view(914 * 4, (4, 16), F32).v())
    cp(lsT.v(), csl(978, 994))
    gin = din("gT", [P, 7 * KC])
    dma("sp", gT.v(), gin.rearrange("p (a n) -> p a n", a=7))

    xT = din("xT", [D, T])
    xh = din("xhT", [D, TH])
    xv = xT.rearrange("(c p) t -> p c t", p=P)
    for c0 in range(0, KC, 4):
        dma("sp", hT.v((slice(c0, c0 + 4), slice(0, T))), xv[:, c0:c0 + 4, :])
    dma("sp", hT.v((slice(0, KC), slice(T, TT))), xh.rearrange("(c p) t -> p c t", p=P))

    cTin = din("cT", [P, KC * 4])
    awin = din("aw", [18 * D, 256])
    abin = din("ab", [P, 36])
    scf = A_t.view(8192, (KC, 4), F32)
    scb = A_t.view(8192 + 256, (KC, 4), BF16)
    abt = A_t.view(8192 + 512, (36,), F32)
    adaP = A_t.view(8192 + 1024, (36, 4), F32)
    dma("sp", scf.v(), cTin.rearrange("p (k b) -> p k b", b=4))
    dma("sp", abt.v(), abin)
    act(scb.v(), scf.v(), AF.Silu)
    pada = PS(pst[7][:, 0:144].rearrange("p (a b) -> p a b", b=4), 7)
    for lv in range(18):
        wt = loadw(awin[lv * D:(lv + 1) * D, :].rearrange("(k p) n -> p k n", p=P), (KC, 256))
        for j in range(2):
            o = PS(pst[7][:, (lv * 2 + j) * 4:(lv * 2 + j) * 4 + 4], 7)
            for k in range(KC):
                mm(o, wt.v((k, slice(j * 128, (j + 1) * 128))), scb.v((k,)), k == 0, k == KC - 1)
    sc.add("dve", lambda e: e.tensor_tensor(adaP.ap, pada.ap, abt.ap.unsqueeze(2).to_broadcast([P, 36, 4]), ALU.add),
           reads=(pada, abt.v()), writes=(adaP.v(),))
    ada_in = nc.dram_tensor("ada_in", [P, 144], F32).ap()
    ada_out = nc.dram_tensor("ada_out", [8 * P, 144], F32).ap()
    dflag = View(None, "dramflag", [(0, 1)])
    sc.add("pool", lambda e: e.dma_start(out=ada_in, in_=adaP.ap.rearrange("p a b -> p (a b)")),
           reads=(adaP.v(),), writes=(dflag,), kind="d")
    sc.add("pool", lambda e: e.collective_compute("AllGather", ALU.bypass, replica_groups=[list(range(8))],
                                                  ins=[ada_in.opt()], outs=[ada_out.opt()]),
           reads=(dflag,), writes=(dflag,), kind="cc")
    adaG = A_t.view(0, (8, 36, 4), F32)
    sc.add("pool", lambda e: e.dma_start(out=adaG.ap, in_=ada_out.rearrange("(r p) (a b) -> p r a b", p=P, b=4)),
           reads=(dflag, ), writes=(adaG.v(),), kind="d")
    adaM = A_t.view(4608, (8, 36, 4), F32)
    adaS = A_t.view(9216, (8, 36), F32)
    sel = cpk.v((slice(4, 8),))
    sc.add("dve", lambda e: e.tensor_tensor(adaM.ap, adaG.ap, sel.ap.unsqueeze(1).unsqueeze(1).to_broadcast([P, 8, 36, 4]), ALU.mult),
           reads=(adaG.v(), sel), writes=(adaM.v(),))
    sc.add("dve", lambda e: e.tensor_reduce(adaS.ap, adaM.ap, AX.X, ALU.add), reads=(adaM.v(),), writes=(adaS.v(),))
    for l in range(2):
        for s in range(3):
            base = (l * 3 + s) * 3

            def adav(k, l=l, s=s):
                a0 = (l * 9 + s * 3 + k) * 2
                return View(adaS.ap[:, :, a0:a0 + 2], A_t, adaS.v().ranges)
            gv = View(gT.ap[:, l * 3 + s, :].rearrange("p (r j) -> p r j", j=2), A_m, gT.v((l * 3 + s,)).ranges)

            def mv(i):
                return View(modT.ap[:, base + i, :].rearrange("p (r j) -> p r j", j=2), A_m, modT.v((base + i,)).ranges)
            stt(mv(0), adav(1), 1.0, gv, ALU.add, ALU.mult)
            cp(mv(1), adav(0))
            ts(mv(2), adav(2), 0.5 if s != 1 else 1.0, None, ALU.mult)

    def modv(l, s, i, c):
        return modT.v(((l * 3 + s) * 3 + i, slice(c, c + 1)))

    rstd = smallw

    def norm_mod(l, s, tchunks, final=False):
        for (t0, n) in tchunks:
            pn = ps(6, n)
            for c in range(KC):
                sq = stg.v((c % 2, slice(0, n)))
                act(sq, hT.v((c, slice(t0, t0 + n))), AF.Square)
                mm(pn, onesb.v(), sq, c == 0, c == KC - 1)
            r0 = rstd.v((slice(0, n),))
            act(r0, pn, AF.Sqrt, bias=EPS_T.v(), scale=1.0 / D)
            sc.add("dve", lambda e, o=r0.ap: e.reciprocal(o, o), reads=(r0,), writes=(r0,))
            for c in range(KC):
                hv = hT.v((c, slice(t0, t0 + n)))
                if final:
                    tmp = tmpA.v((c % 2, slice(0, n)))
                    tt(tmp, hv, r0, ALU.mult)
                    act(hv, tmp, AF.Identity, scale=gT.v((6, slice(c, c + 1))))
                else:
                    tmp = tmpA.v((c % 2, slice(0, n)))
                    tt(tmp, hv, r0, ALU.mult)
                    act(yT.v((c, slice(t0, t0 + n))), tmp, AF.Identity, bias=modv(l, s, 1, c), scale=modv(l, s, 0, c))

    EPS_T = malloc((1,), F32)
    sc.add("dve", lambda e: e.memset(EPS_T.ap, EPS), writes=(EPS_T.v(),))

    def resid_add(l, s, c, t0, n, pso):
        hv = hT.v((c, slice(t0, t0 + n)))
        stt(hv, pso, modv(l, s, 2, c), hv, ALU.mult, ALU.add)

    def ffn(l, j, tchunks):
        s = 0 if j == 0 else 2
        wgu = din("ffn_wgu", [2, 2, D, 2 * DFF])[l, j].rearrange("(k p) n -> p k n", p=P)
        wd = din("ffn_wd", [2, 2, DFF, D])[l, j]
        pi = [0]
        for (f0, nf) in FPARTS:
            for pr in range(nf // 2):
                fa = f0 + pr * 2
                wg = loadw(wgu[:, :, fa * 128:fa * 128 + 256], (KC, 256))
                wu = loadw(wgu[:, :, DFF + fa * 128:DFF + fa * 128 + 256], (KC, 256))
                for fc in range(2):
                    fl = fa + fc - f0
                    for (t0, n) in tchunks:
                        b = pi[0] % 2
                        pi[0] += 1
                        pg = ps(b * 2, n)
                        pu = ps(b * 2 + 1, n)
                        for k in range(KC):
                            mm(pg, wg.v((k, slice(fc * 128, fc * 128 + 128))), yT.v((k, slice(t0, t0 + n))), k == 0, k == KC - 1)
                        for k in range(KC):
                            mm(pu, wu.v((k, slice(fc * 128, fc * 128 + 128))), yT.v((k, slice(t0, t0 + n))), k == 0, k == KC - 1)
                        sg = tmpA.v((2 + b, slice(0, n)))
                        act(sg, pg, AF.Silu)
                        tt(bT.v((fl, slice(t0, t0 + n))), sg, pu, ALU.mult)
            for dp in range(8):
                wdt = loadw(wd[f0 * 128:(f0 + nf) * 128, dp * 256:(dp + 1) * 256].rearrange("(k p) n -> p k n", p=P), (nf, 256))
                for dc in range(2):
                    c = dp * 2 + dc
                    for (t0, n) in tchunks:
                        b = pi[0] % 2
                        pi[0] += 1
                        po = ps(4 + b, n)
                        for k in range(nf):
                            mm(po, wdt.v((k, slice(dc * 128, dc * 128 + 128))), bT.v((k, slice(t0, t0 + n))), k == 0, k == nf - 1)
                        resid_add(l, s, c, t0, n, po)

    def proj_resid(wv2d, src, l, s, tchunks):
        wv = wv2d.rearrange("(k p) n -> p k n", p=P)
        pi = 0
        for dp in range(8):
            wt = loadw(wv[:, :, dp * 256:(dp + 1) * 256], (KC, 256))
            for dc in range(2):
                c = dp * 2 + dc
                for (t0, n) in tchunks:
                    po = ps(4 + pi % 2, n)
                    pi += 1
                    for k in range(KC):
                        mm(po, wt.v((k, slice(dc * 128, dc * 128 + 128))), src.v((k, slice(t0, t0 + n))), k == 0, k == KC - 1)
                    resid_add(l, s, c, t0, n, po)

    def pool_mixer2(l):
        w_in = din("pool_w_in", [1, D, D])[0].rearrange("(k p) n -> p k n", p=P)
        w_grp = din("pool_w_grp", [1, 4, 512, 512])[0]
        w_out = din("pool_w_out", [1, D, D])[0]
        u0 = A_t.view(0, (TT,), F32)
        sA = A_t.view(TT * 4, (TT,), F32)
        sB = A_t.view(2 * TT * 4, (TT,), F32)
        pT = bT
        flag = cpk.v((slice(0, 1),))
        for dp in range(8):
            wt = loadw(w_in[:, :, dp * 256:(dp + 1) * 256], (KC, 256))
            for dc in range(2):
                c = dp * 2 + dc
                g = c // 4
                w = 2 ** (g + 1)
                for ti, (t0, n) in enumerate(WITH_HALO):
                    po = ps(ti % 2, n)
                    for k in range(KC):
                        mm(po, wt.v((k, slice(dc * 128, dc * 128 + 128))), yT.v((k, slice(t0, t0 + n))), k == 0, k == KC - 1)
                    if t0 == T:
                        act(u0.v((slice(0, TH),)), po, AF.Identity, scale=flag)
                    else:
                        act(u0.v((slice(TH + t0, TH + t0 + n),)), po, AF.Identity)
                src, step = u0, 1
                dsts = [sA, sB]
                di = 0
                while step < w:
                    lo = 2 * step
                    dst = dsts[di]
                    di = 1 - di
                    tt(dst.v((slice(lo, TT),)), src.v((slice(lo, TT),)), src.v((slice(lo - step, TT - step),)), ALU.add)
                    src, step = dst, step * 2
                stt(pT.v((c, slice(0, T))), src.v((slice(TH, TT),)), 1.0 / w, u0.v((slice(TH, TT),)), ALU.mult, ALU.subtract)
                t16 = dsts[di].v((slice(0, 16),))
                tt(t16, src.v((slice(TH, TH + 16),)), invc.v((g,)), ALU.mult)
                tt(pT.v((c, slice(0, 16))), t16, u0.v((slice(TH, TH + 16),)), ALU.subtract)
        pi = 0
        for g in range(4):
            for hp in range(2):
                wt = loadw(w_grp[g, :, hp * 256:(hp + 1) * 256].rearrange("(k p) n -> p k n", p=P), (4, 256))
                for dc in range(2):
                    c = g * 4 + hp * 2 + dc
                    for (t0, n) in MAIN:
                        po = ps(pi % 2, n)
                        pi += 1
                        for k in range(4):
                            mm(po, wt.v((k, slice(dc * 128, dc * 128 + 128))), pT.v((g * 4 + k, slice(t0, t0 + n))), k == 0, k == 3)
                        act(yT.v((c, slice(t0, t0 + n))), po, AF.Identity, scale=lsT.v((slice(c, c + 1),)))
        proj_resid(w_out, yT, l, 1, MAIN)

    def dsa_mixer(l):
        w_in = din("dsa_w_in", [1, D, 4176])[0].rearrange("(k p) n -> p k n", p=P)
        w_out = din("dsa_w_out", [1, D, D])[0]
        pos = din("pos", [P, T], I32)
        qT = bT
        RT = A_w.view(3 * SLOT * 2, (4, T), F32)
        pi32 = A_t.view(0, (T,), I32)
        pf = A_t.view(4096, (T,), F32)
        ang = A_t.view(8192, (T,), F32)
        kk = A_t.view(12288, (T,), I32)
        sc.add("sp", lambda e: e.dma_start(out=pi32.ap, in_=pos), writes=(pi32.v(),), kind="d")
        cp(pf.v(), pi32.v())
        TWO_PI = 2.0 * math.pi
        for var in range(2):
            for cs_i, shift in enumerate((math.pi / 2, 0.0)):
                dstt = RT.v((var * 2 + cs_i,))
                ts(ang.v(), pf.v(), frq.v((slice(var, var + 1),)), shift, ALU.mult, ALU.add)
                ts(kk.v(), ang.v(), 1.0 / TWO_PI, None, ALU.mult)
                cp(dstt, kk.v())
                stt(ang.v(), dstt, -TWO_PI, ang.v(), ALU.mult, ALU.add)
                ts(ang.v(), ang.v(), 3.14159, -3.14159, ALU.min, ALU.max)
                act(dstt, ang.v(), AF.Sin)
        wslot[0] = 0
        NS_SAVE = 3

        def loadw4(dram_view, shape):
            n = int(np.prod(shape))
            s_ = wslot[0] % NS_SAVE
            wslot[0] += 1
            t = A_w.view(s_ * SLOT * 2, shape, BF16)
            dma("pool", t.v(), dram_view)
            return t

        kx_in = nc.dram_tensor("kx_in", [576, T], BF16).ap()
        kx_out = nc.dram_tensor("kx_out", [2 * 576, T], BF16).ap()
        vx_in = nc.dram_tensor("vx_in", [T, 512], BF16).ap()
        vx_out = nc.dram_tensor("vx_out", [2 * T, 512], BF16).ap()
        kxf = View(None, "kxflag", [(0, 1)])
        vxf = View(None, "vxflag", [(0, 1)])

        x32b = [A_t.view(0, (512,), F32), A_t.view(2048, (512,), F32)]
        ui = [0]

        def rope_chunk(pso, npart, var, t0, dst_view):
            i = ui[0] % 2
            ui[0] += 1
            x32 = View(XR[i].ap[0:npart, :], A_m, XR[i].v().ranges)
            cp(x32, pso, eng="act" if False else "dve")
            psw = pswq if var == 0 else pswi
            pw = PS(pst[6][0:npart, 0:512], 6)
            mm(pw, View(psw.ap[0:npart, 0:npart], A_m, psw.v().ranges), x32, True, True)
            cosv = View(RT.ap[0:npart, var * 2, t0:t0 + 512], A_w, RT.v((var * 2, slice(t0, t0 + 512))).ranges)
            sinv = View(RT.ap[0:npart, var * 2 + 1, t0:t0 + 512], A_w, RT.v((var * 2 + 1, slice(t0, t0 + 512))).ranges)
            t1 = View(XS[i].ap[0:npart, :], A_m, XS[i].v().ranges)
            tt(t1, pw, sinv, ALU.mult)
            tt(x32, x32, cosv, ALU.mult)
            tt(dst_view, x32, t1, ALU.add)

        XR0 = malloc((512,), F32)
        XR = [XR0, XR0]
        XS0 = smallw.arena.view(smallw.off, (512,), F32)
        XS = [XS0, XS0]

        pj = [0]

        def nextps():
            b = pj[0] % 2
            pj[0] += 1
            return b
        for hp in range(8):
            wt = loadw4(w_in[:, :, hp * 256:(hp + 1) * 256], (KC, 256))
            for hc in range(2):
                h = hp * 2 + hc
                for (t0, n) in MAIN:
                    po = ps(nextps(), n)
                    for k in range(KC):
                        mm(po, wt.v((k, slice(hc * 128, hc * 128 + 128))), yT.v((k, slice(t0, t0 + n))), k == 0, k == KC - 1)
                    rope_chunk(po, 128, 0, t0, qT.v((h, slice(t0, t0 + n))))
        for hp in range(2):
            wt = loadw4(w_in[:, :, 2048 + hp * 256:2048 + (hp + 1) * 256], (KC, 256))
            for hc in range(2):
                g = hp * 2 + hc
                for (t0, n) in MAIN:
                    po = ps(nextps(), n)
                    for k in range(KC):
                        mm(po, wt.v((k, slice(hc * 128, hc * 128 + 128))), yT.v((k, slice(t0, t0 + n))), k == 0, k == KC - 1)
                    st = stg.v((ui[0] % 2,))
                    rope_chunk(po, 128, 0, t0, st)
                    sc.add("sp", lambda e, o=kx_in[g * 128:(g + 1) * 128, t0:t0 + n], i=st.ap: e.dma_start(out=o, in_=i),
                           reads=(st,), writes=(kxf,), kind="d")
        for hp in range(2):
            wt = loadw4(w_in[:, :, 2560 + hp * 256:2560 + (hp + 1) * 256], (KC, 256))
            for tb in range(8):
                po = ps(nextps(), 256)
                for k in range(KC):
                    mm(po, yT.v((k, slice(tb * 128, tb * 128 + 128))), wt.v((k,)), k == 0, k == KC - 1)
                st = stg.v((tb % 2, slice(0, 256)))
                cp(st, po)
                sc.add("sp", lambda e, o=vx_in[tb * 128:(tb + 1) * 128, hp * 256:(hp + 1) * 256], i=st.ap: e.dma_start(out=o, in_=i),
                       reads=(st,), writes=(vxf,), kind="d")
        wt = loadw4(w_in[:, :, 4096:4176], (KC, 80))
        for (t0, n) in MAIN:
            po = ps(nextps(), n, p=(0, 64))
            for k in range(KC):
                mm(po, wt.v((k, slice(0, 64))), yT.v((k, slice(t0, t0 + n))), k == 0, k == KC - 1)
            stt_ = stg.v((ui[0] % 2,))
            st = View(stt_.ap[0:64, :], A_m, stt_.ranges)
            rope_chunk(po, 64, 1, t0, st)
            sc.add("sp", lambda e, o=kx_in[512:576, t0:t0 + n], i=st.ap: e.dma_start(out=o, in_=i),
                   reads=(st,), writes=(kxf,), kind="d")
        for tb in range(8):
            po = ps(nextps(), 16)
            for k in range(KC):
                mm(po, yT.v((k, slice(tb * 128, tb * 128 + 128))), wt.v((k, slice(64, 80))), k == 0, k == KC - 1)
            cp(wiT.v((tb,)), po)
        sc.add("pool", lambda e: e.collective_compute("AllGather", ALU.bypass, replica_groups=[[0, 1], [2, 3], [4, 5], [6, 7]],
                                                      ins=[kx_in.opt()], outs=[kx_out.opt()]),
               reads=(kxf,), writes=(kxf,), kind="cc")
        sc.add("pool", lambda e: e.collective_compute("AllGather", ALU.bypass, replica_groups=[[0, 1], [2, 3], [4, 5], [6, 7]],
                                                      ins=[vx_in.opt()], outs=[vx_out.opt()]),
               reads=(vxf,), writes=(vxf,), kind="cc")
        for hp in range(4):
            wt = loadw4(w_in[:, :, 3072 + hp * 256:3072 + (hp + 1) * 256], (KC, 256))
            for hc in range(2):
                c = hp * 2 + hc
                for (t0, n) in MAIN:
                    po = ps(nextps(), n)
                    for k in range(KC):
                        mm(po, wt.v((k, slice(hc * 128, hc * 128 + 128))), yT.v((k, slice(t0, t0 + n))), k == 0, k == KC - 1)
                    rope_chunk(po, 128, 1, t0, qiT.v((c, slice(t0, t0 + n))))
        kTf = A_w.view(0, (4, S), BF16)
        Vf = A_w.view(16384, (16, 512), BF16)
        ki2 = A_w.view(32768, (S,), BF16)
        mb = A_w.view(36864, (S,), BF16)
        ptb = [A_w.view(40960 + i * 1024, (512,), BF16) for i in range(3)]
        Rr = [A_y.view(28672 + i * 2048, (512,), F32) for i in range(2)]
        kxo = kx_out.rearrange("(r x) s -> x r s", r=2)
        for g in range(4):
            sc.add("sp", lambda e, o=kTf.ap[:, g, :].rearrange("p (r s) -> p r s", r=2), i=kxo[g * 128:(g + 1) * 128]: e.dma_start(out=o, in_=i),
                   reads=(kxf,), writes=(kTf.v((g,)),), kind="d")
        for hh in range(2):
            kv = View(ki2.ap[hh * 64:(hh + 1) * 64, :].rearrange("p (r s) -> p r s", r=2), A_w, ki2.v().ranges)
            sc.add("sp", lambda e, o=kv.ap, i=kxo[512:576]: e.dma_start(out=o, in_=i), reads=(kxf,), writes=(kv,), kind="d")
        sc.add("sp", lambda e: e.dma_start(out=Vf.ap, in_=vx_out.rearrange("(b p) f -> p b f", p=P)), reads=(vxf,), writes=(Vf.v(),), kind="d")
        accs = [A_y.view(0, (S,), F32), A_y.view(8192, (S,), F32)]
        Wk = A_y.view(16384, (S,), F32)
        iot = A_y.view(24576, (S,), I16)
        mbs = [mb, A_y.view(28672, (S,), BF16)]
        ksq = A_m.view(smallw.off, (512,), BF16)
        tmpR = A_m.view(XR0.off, (512,), F32)
        Rr = [A_m.view(stg.off, (512,), BF16), A_m.view(stg.off + 1024, (512,), BF16)]
        mt = malloc((128,), F32, align=512)
        m8 = A_m.view(mt.off, (8,), F32)
        thr = A_m.view(mt.off + 32, (2,), F32)
        negb = malloc((512,), BF16, align=512)
        sc.add("pool", lambda e: e.iota(iot.ap, [[1, S]], base=0, channel_multiplier=0, allow_small_or_imprecise_dtypes=True),
               reads=(), writes=(iot.v(),))
        for i in range(8):
            ts(rp.v((slice(i, i + 1),)), cpk.v((slice(1, 2),)), float(i * 128), None, ALU.add)
        for g in range(4):
            for sb4 in range(4):
                kq = ksq.v()
                act(kq, kTf.v((g, slice(sb4 * 512, sb4 * 512 + 512))), AF.Square)
                pk = PS(pst[6][0:1, 0:512], 6)
                mm(pk, View(onesb.ap[:, 0:1], A_m, onesb.v().ranges), kq, True, True)
                dstk = View(smallw.ap[0:1, 256 + g * 4 + sb4:256 + g * 4 + sb4 + 1], A_m, smallw.v((slice(256, 512),)).ranges)
                sc.add("dve", lambda e, o=dstk.ap, i=pk.ap: e.tensor_reduce(o, i, AX.X, ALU.max), reads=(pk,), writes=(dstk,))
            src4 = View(smallw.ap[0:1, 256 + g * 4:256 + g * 4 + 4], A_m, smallw.v((slice(256, 512),)).ranges)
            kd = View(kmx.ap[0:1, g:g + 1], A_m, kmx.v().ranges)
            sc.add("dve", lambda e, o=kd.ap, i=src4.ap: e.tensor_reduce(o, i, AX.X, ALU.max), reads=(src4,), writes=(kd,))
        km4 = View(kmx.ap[0:1, 0:4], A_m, kmx.v().ranges)
        km4s = View(kmx.ap[0:1, 4:8], A_m, kmx.v().ranges)
        act(km4s, km4, AF.Sqrt)
        ts(km4s, km4s, -1.03, None, ALU.mult)
        SCALE = 1.0 / math.sqrt(128.0)
        def stageA(i):
            nkb = 9 + i
            nk = nkb * 128
            acc = accs[i % 2]
            ts(acc.v((slice(0, nk),)), iot.v((slice(0, nk),)), rp.v((slice(i, i + 1),)), NEG, ALU.is_gt, ALU.mult)
            ri = 0
            for h in range(16):
                c, pb = h // 2, (h % 2) * 64
                k0 = 0
                while k0 < nk:
                    n = min(512, nk - k0)
                    b = nextps()
                    px = ps(b, n)
                    lh = View(qiT.ap[pb:pb + 64, c, i * 128:(i + 1) * 128], A_t, qiT.v((c, slice(i * 128, (i + 1) * 128))).ranges)
                    rh = View(ki2.ap[pb:pb + 64, k0:k0 + n], A_w, ki2.v((slice(k0, k0 + n),)).ranges)
                    mm(px, lh, rh, True, True)
                    rr = Rr[ri % 2].v((slice(0, n),))
                    ri += 1
                    act(rr, px, AF.Relu)
                    av = acc.v((slice(k0, k0 + n),))
                    stt(av, rr, wiT.v((i, slice(h, h + 1))), av, ALU.mult, ALU.add)
                    k0 += n

        def stageB(i):
            nkb = 9 + i
            nk = nkb * 128
            acc = accs[i % 2]
            mbi = mbs[i % 2]
            for r in range(32):
                srcv = acc.v((slice(0, nk),)) if r == 0 else Wk.v((slice(0, nk),))
                sc.add("dve", lambda e, o=m8.ap, s_=srcv.ap: e.max(out=o, in_=s_), reads=(srcv,), writes=(m8.v(),))
                if r < 31:
                    wv = Wk.v((slice(0, nk),))
                    sc.add("dve", lambda e, o=wv.ap, s_=srcv.ap, m=m8.ap: e.match_replace(out=o, in_to_replace=m, in_values=s_, imm_value=-3.0e38),
                           reads=(srcv, m8.v()), writes=(wv,))
            ts(thr.v((slice(0, 1),)), m8.v((slice(7, 8),)), -1.0e29, None, ALU.max)
            ts(mbi.v((slice(0, nk),)), acc.v((slice(0, nk),)), thr.v((slice(0, 1),)), NEGB, ALU.is_lt, ALU.mult)

        def stageC(i):
            nkb = 9 + i
            mbi = mbs[i % 2]
            for g in range(4):
                qblk = View(qT.ap[:, 4 * g:4 * g + 4, i * 128:(i + 1) * 128], A_b,
                            qT.v((slice(4 * g, 4 * g + 4), slice(i * 128, (i + 1) * 128))).ranges)
                sq4 = View(ksq.ap.rearrange("p (a b) -> p a b", a=4), A_m, ksq.v().ranges)
                act(sq4, qblk, AF.Square)
                pq = PS(pst[6][0:1, 0:512], 6)
                mm(pq, View(onesb.ap[:, 0:1], A_m, onesb.v().ranges), ksq.v(), True, True)
                nb0 = View(smallw.ap[0:1, 512:1024], A_m, smallw.v((slice(512, 1024),)).ranges)
                act(nb0, pq, AF.Sqrt)
                nbv = View(negb.ap[0:1, :], A_m, negb.v().ranges)
                ts(nbv, nb0, View(kmx.ap[0:1, 4 + g:5 + g], A_m, kmx.v().ranges), None, ALU.mult)
                po = ps(4)
                pd = ps(5)
                for sb in range(nkb):
                    pS = ps(2 + sb % 2)
                    mm(pS, kTf.v((g, slice(sb * 128, sb * 128 + 128))), qblk, True, False)
                    mm(pS, mbi.v((slice(sb * 128, sb * 128 + 128),)), ident4.v(), False, False)
                    mm(pS, View(onesb.ap[0:1, :], A_m, onesb.v().ranges), nbv, False, True)
                    pt = ptb[sb % 3].v()
                    act(pt, pS, AF.Exp, scale=SCALE)
                    mm(po, Vf.v((sb, slice(g * 128, g * 128 + 128))), pt, sb == 0, sb == nkb - 1)
                    mm(pd, onesb.v(), pt, sb == 0, sb == nkb - 1)
                rd = tmpR.v()
                sc.add("dve", lambda e, o=rd.ap, i_=pd.ap: e.reciprocal(o, i_), reads=(pd,), writes=(rd,))
                ov = View(qblk.ap, A_b, qblk.ranges)
                sc.add("dve", lambda e, o=ov.ap, a=po.ap.rearrange("p (a b) -> p a b", a=4), b_=rd.ap.rearrange("p (a b) -> p a b", a=4):
                       e.tensor_tensor(o, a, b_, ALU.mult), reads=(po, rd), writes=(ov,))

        def record(fn, *args):
            ops_ = []
            orig = sc.add
            sc.add = lambda *a, **k: ops_.append((a, k))
            try:
                fn(*args)
            finally:
                sc.add = orig
            return ops_

        def merge_emit(streams):
            streams = [s_ for s_ in streams if s_]
            total = max(len(s_) for s_ in streams)
            idx = [0] * len(streams)
            for step in range(total):
                for si_, s_ in enumerate(streams):
                    target = (step + 1) * len(s_) // total
                    while idx[si_] < target:
                        a_, k_ = s_[idx[si_]]
                        sc.add(*a_, **k_)
                        idx[si_] += 1

        stageA(0)
        for st_ in range(8):
            streams = [record(stageB, st_)]
            if st_ + 1 < 8:
                streams.append(record(stageA, st_ + 1))
            if st_ >= 1:
                streams.append(record(stageC, st_ - 1))
            merge_emit(streams)
        stageC(7)
        wslot[0] = 0
        proj_resid(w_out, qT, l, 1, MAIN)


    stages = ["ffn1_0", "pool", "ffn2_0", "ffn1_1", "dsa", "full"]
    if stop == "pool_only":
        norm_mod(0, 1, WITH_HALO)
        pool_mixer2(0)
    elif stop == "dsa_only":
        norm_mod(1, 1, MAIN)
        dsa_mixer(1)
    else:
        si = stages.index(stop)
        norm_mod(0, 0, WITH_HALO)
        ffn(0, 0, WITH_HALO)
        if si >= 1:
            norm_mod(0, 1, WITH_HALO)
            pool_mixer2(0)
        if si >= 2:
            norm_mod(0, 2, MAIN)
            ffn(0, 1, MAIN)
        if si >= 3:
            norm_mod(1, 0, MAIN)
            ffn(1, 0, MAIN)
        if si >= 4:
            norm_mod(1, 1, MAIN)
            dsa_mixer(1)
        if si >= 5:
            norm_mod(1, 2, MAIN)
            ffn(1, 1, MAIN)
            norm_mod(0, 0, MAIN, final=True)
    ov = outT.rearrange("(c p) t -> p c t", p=P)
    for c0 in range(0, KC, 4):
        dma_out("sp", ov[:, c0:c0 + 4, :], hT.v((slice(c0, c0 + 4), slice(0, T))))
    last = [op for op in sc.q["sp"] if op["kind"] == "d"][-4:]
    sc.finalize()
    sems = {}
    for e in Sched.COMPUTE:
        sems[("c", e)] = nc.alloc_semaphore(f"c_{e}")
    for e in ("sp", "pool"):
        for i in range(sc.nring):
            sems[("d", e, i)] = nc.alloc_semaphore(f"d_{e}_{i}")
    sems[("cc",)] = nc.alloc_semaphore("ccs")
    with nc.Block() as block:
        def final_wait(e):
            for op in last:
                e.wait_ge(sems[op["sem"]], op["val"])
            return e.nop()
        fop = dict(id=-1, eng="sp", fn=final_wait, kind="c", deps=set(), need=[], signal=False, idx=len(sc.q["sp"]))
        sc.q["sp"].append(fop)
        sc.emit(nc, block, sems)
    return nc, list(dram.keys())


def _consts(hf):
    cst = np.zeros((P, 1024), np.float32)
    eye = np.eye(P, dtype=np.float32)
    cst[:, 0:512] = np.tile(eye, (1, 4))
    cst[:, 512:640] = 1.0
    def psw(block, half):
        m = np.zeros((P, P), np.float32)
        for b0 in range(0, P, block):
            for dd in range(half):
                m[b0 + dd + half, b0 + dd] = -1.0
                m[b0 + dd, b0 + dd + half] = 1.0
        return m
    cst[:, 640:768] = psw(128, 16)
    cst[:, 768:896] = psw(64, 8)
    fq = np.zeros(P, np.float32)
    fq[0:16] = THETA ** (-np.arange(16, dtype=np.float32) / 16)
    fq[16:32] = fq[0:16]
    fi = np.zeros(P, np.float32)
    for b0 in (0, 64):
        fi[b0:b0 + 8] = THETA ** (-np.arange(8, dtype=np.float32) / 8)
        fi[b0 + 8:b0 + 16] = fi[b0:b0 + 8]
    cst[:, 896] = fq
    cst[:, 897] = fi
    return cst


def kernel(x, c, positions, ada_w, ada_b, norm_g, final_g, ffn_wgu, ffn_wd,
           pool_w_in, pool_w_grp, pool_scale, pool_w_out, dsa_w_in, dsa_w_out, _stop="full"):
    nc, names = build(_stop)
    x = np.asarray(x, np.float32)
    f = lambda a: np.ascontiguousarray(np.asarray(a))
    gT = np.concatenate([np.asarray(norm_g, np.float32).reshape(6, D), np.asarray(final_g, np.float32).reshape(1, D)], 0)
    gT = f(gT.reshape(7, KC, P).transpose(2, 0, 1).reshape(P, 7 * KC))
    cT = f(np.asarray(c, np.float32).reshape(4, KC, P).transpose(2, 1, 0).reshape(P, KC * 4))
    aw = np.asarray(ada_w, np.float32).reshape(2, D, 9, D)
    ab = np.asarray(ada_b, np.float32).reshape(2, 9, D)
    ls = np.asarray(pool_scale, np.float32).reshape(KC, P).T
    in_maps = []
    for core in range(8):
        b, hf = core // 2, core % 2
        m = {}
        m["xT"] = f(x[b, hf * T:(hf + 1) * T, :].T)
        m["xhT"] = f(x[b, T - TH:T, :].T) if hf == 1 else np.zeros((D, TH), np.float32)
        cst = _consts(hf)
        cst[:, 898] = float(hf)
        cst[:, 899] = np.arange(P, dtype=np.float32) + hf * T
        cst[:, 902 + b] = 1.0
        for g, w in enumerate((2, 4, 8, 16)):
            tpos = np.arange(16) + hf * T
            cst[:, 914 + g * 16:914 + (g + 1) * 16] = (1.0 / np.minimum(tpos + 1, w)).astype(np.float32)[None, :]
        cst[:, 978:994] = ls
        m["cst"] = cst
        m["gT"] = gT
        m["cT"] = cT
        m["aw"] = f(aw[:, :, :, core * 256:(core + 1) * 256].transpose(0, 2, 1, 3).reshape(18 * D, 256))
        m["ab"] = f(ab[:, :, core * 256:(core + 1) * 256].reshape(18, 2, P).transpose(2, 0, 1).reshape(P, 36))
        m["pos"] = f(np.tile(np.asarray(positions, np.int32)[b, hf * T:(hf + 1) * T].reshape(1, T), (P, 1)))
        m["ffn_wgu"] = f(ffn_wgu)
        m["ffn_wd"] = f(ffn_wd)
        m["pool_w_in"] = f(pool_w_in)
        m["pool_w_grp"] = f(pool_w_grp)
        m["pool_w_out"] = f(pool_w_out)
        m["dsa_w_in"] = f(dsa_w_in)
        m["dsa_w_out"] = f(dsa_w_out)
        in_maps.append({k: m[k] for k in names})
    res = run_bass_kernel_spmd(nc, in_maps, core_ids=list(range(8)))
    out = np.zeros((4, S, D), np.float32)
    for core in range(8):
        b, hf = core // 2, core % 2
        out[b, hf * T:(hf + 1) * T, :] = res.results[core]["outT"].T
    return out
```

```python
import math
import numpy as np
import concourse.bass as bass
import concourse.mybir as mybir
from concourse.bass_utils import run_bass_kernel_spmd

F32 = mybir.dt.float32
BF16 = mybir.dt.bfloat16
I32 = mybir.dt.int32
I16 = mybir.dt.int16
ALU = mybir.AluOpType
AF = mybir.ActivationFunctionType
AX = mybir.AxisListType

P = 128
D = 2048
KC = 16
T = 1024
TH = 16
TT = T + TH
DFF = 5632
S = 2048
NEG = -1.0e30
NEGB = -30000.0
EPS = 1e-6
THETA = 500000.0
MAIN = [(0, 512), (512, 512)]
WITH_HALO = MAIN + [(T, TH)]
HALO3 = [(0, 352), (352, 352), (704, 336)]
FPARTS = [(0, 12), (12, 12), (24, 10), (34, 10)]
SLOT = 4096
NSLOT = 5
ESZ = {F32: 4, BF16: 2, I32: 4, I16: 2}


class View:
    def __init__(self, ap, arena, ranges):
        self.ap = ap
        self.arena = arena
        self.ranges = ranges


class Arena:
    def __init__(self, name, tensor, nbytes):
        self.name = name
        self.t = tensor
        self.nbytes = nbytes

    def view(self, off, shape, dt):
        e = ESZ[dt]
        n = int(np.prod(shape))
        assert off % 4 == 0 and off + n * e <= self.nbytes, (self.name, off, shape)
        ap = self.t[:, off // 2:(off + n * e) // 2]
        if dt != BF16:
            ap = ap.bitcast(dt)
        if len(shape) == 2:
            ap = ap.rearrange("p (a b) -> p a b", a=shape[0])
        elif len(shape) == 3:
            ap = ap.rearrange("p (a b c) -> p a b c", a=shape[0], b=shape[1])
        return Tile(self, off, tuple(shape), dt, ap)


class Tile:
    def __init__(self, arena, off, shape, dt, ap):
        self.arena, self.off, self.shape, self.dt, self.ap = arena, off, shape, dt, ap

    def _norm(self, idx):
        if not isinstance(idx, tuple):
            idx = (idx,)
        idx = list(idx) + [slice(None)] * (len(self.shape) - len(idx))
        out = []
        for i, s in zip(idx, self.shape):
            if isinstance(i, int):
                out.append((i, i + 1, True))
            else:
                a = 0 if i.start is None else i.start
                b = s if i.stop is None else i.stop
                out.append((a, b, False))
        return out

    def v(self, idx=(), p=None):
        nidx = self._norm(idx)
        e = ESZ[self.dt]
        strides = []
        acc = 1
        for s in reversed(self.shape):
            strides.append(acc)
            acc *= s
        strides = list(reversed(strides))
        ranges = []

        def rec(d, base):
            if d == len(nidx) - 1:
                a, b, _ = nidx[d]
                ranges.append((self.off + (base + a) * e, self.off + (base + b) * e))
                return
            a, b, _ = nidx[d]
            for i in range(a, b):
                rec(d + 1, base + i * strides[d])
        rec(0, 0)
        key = tuple(slice(a, b) if not isint else a for (a, b, isint) in nidx)
        pk = slice(None) if p is None else slice(p[0], p[1])
        ap = self.ap[(pk,) + key]
        return View(ap, self.arena, ranges)


class PS:
    def __init__(self, ap, bank):
        self.ap = ap
        self.arena = "psum"
        self.ranges = [(bank, bank + 1)]


BLK = 512


class Sched:
    COMPUTE = ("pe", "act", "dve", "pool")

    def __init__(self):
        self.ops = []
        self.q = {e: [] for e in ("pe", "act", "dve", "pool", "sp")}
        self.lastw = {}
        self.readers = {}
        self.known = {e: {} for e in self.q}

    def _blocks(self, views):
        out = set()
        for v in views:
            if v is None:
                continue
            name = v.arena if isinstance(v.arena, str) else v.arena.name
            for lo, hi in v.ranges:
                if name == "psum":
                    out.add((name, lo))
                else:
                    for b in range(lo // BLK, (hi - 1) // BLK + 1):
                        out.add((name, b))
        return out

    def add(self, eng, fn, reads=(), writes=(), kind="c"):
        oid = len(self.ops)
        rb = self._blocks(reads)
        wb = self._blocks(writes)
        deps = set()
        for b in rb:
            if b in self.lastw:
                deps.add(self.lastw[b])
        for b in wb:
            if b in self.lastw:
                deps.add(self.lastw[b])
            for r in self.readers.get(b, ()):
                deps.add(r)
        deps.discard(oid)
        op = dict(id=oid, eng=eng, fn=fn, kind=kind, deps=deps, signal=False, idx=len(self.q[eng]))
        for b in rb:
            self.readers.setdefault(b, []).append(oid)
        for b in wb:
            self.lastw[b] = oid
            self.readers[b] = []
        self.ops.append(op)
        self.q[eng].append(op)
        return oid

    def finalize(self, nring=12):
        for op in self.ops:
            eng = op["eng"]
            need = []
            best = {}
            for d in op["deps"]:
                dop = self.ops[d]
                if dop["kind"] == "c":
                    if dop["eng"] == eng and eng in ("pe", "sp"):
                        continue
                    k = dop["eng"]
                    if k not in best or dop["idx"] > best[k]["idx"]:
                        best[k] = dop
                else:
                    need.append(dop)
            for k, dop in best.items():
                if self.known[eng].get(k, -1) >= dop["idx"]:
                    continue
                self.known[eng][k] = dop["idx"]
                need.append(dop)
            op["need"] = need
            for dop in need:
                dop["signal"] = True
        cnt = {e: 0 for e in self.COMPUTE}
        dcount = {e: 0 for e in self.q}
        ccount = 0
        for op in self.ops:
            if op["kind"] == "c":
                if op["signal"]:
                    cnt[op["eng"]] += 1
                    op["sem"] = ("c", op["eng"])
                    op["val"] = cnt[op["eng"]]
            elif op["kind"] == "d":
                e = op["eng"]
                n = dcount[e]
                dcount[e] += 1
                op["sem"] = ("d", e, n % nring)
                op["val"] = 16 * (n // nring + 1)
                op["prev"] = (("d", e, n % nring), 16 * (n // nring)) if n >= nring else None
            else:
                ccount += 1
                op["sem"] = ("cc",)
                op["val"] = ccount
        self.nring = nring

    def emit(self, nc, block, sems):
        engs = {"pe": block.tensor, "act": block.scalar, "dve": block.vector, "pool": block.gpsimd, "sp": block.sync}
        for ename, deco in engs.items():
            ops = self.q[ename]
            if not ops:
                continue

            def body(e, ops=ops):
                for op in ops:
                    waits = []
                    for dop in op["need"]:
                        waits.append((dop["sem"], dop["val"]))
                    if op["kind"] == "d" and op.get("prev") is not None:
                        waits.append(op["prev"])
                    for sk, val in waits:
                        e.wait_ge(sems[sk], val)
                    ins = op["fn"](e)
                    if op["kind"] == "d":
                        ins.then_inc(sems[op["sem"]], 16)
                    elif op["kind"] == "cc":
                        ins.then_inc(sems[op["sem"]])
                    elif op["signal"]:
                        ins.then_inc(sems[op["sem"]], 1)
            deco(body)


def build(stop="full"):
    nc = bass.Bass("TRN2", target_bir_lowering=False)
    sc = Sched()
    dram = {}

    def din(name, shape, dt=F32):
        if name not in dram:
            dram[name] = nc.dram_tensor(name, list(shape), dt, kind="ExternalInput").ap()
        return dram[name]

    outT = nc.dram_tensor("outT", [D, T], F32, kind="ExternalOutput").ap()

    NB_H = KC * TT * 4
    NB_Y = KC * TT * 2
    NB_W = 44 * 1024
    NB_T = 16 * 1024
    NB_M = 16 * 1024
    th = nc.alloc_sbuf_tensor("R_h", [P, NB_H // 2], BF16)
    ty = nc.alloc_sbuf_tensor("R_y", [P, NB_Y // 2], BF16)
    tb = nc.alloc_sbuf_tensor("R_b", [P, NB_Y // 2], BF16)
    tw = nc.alloc_sbuf_tensor("R_w", [P, NB_W // 2], BF16)
    ttm = nc.alloc_sbuf_tensor("R_t", [P, NB_T // 2], BF16)
    tm = nc.alloc_sbuf_tensor("R_m", [P, NB_M // 2], BF16)
    A_h = Arena("h", th, NB_H)
    A_y = Arena("y", ty, NB_Y)
    A_b = Arena("b", tb, NB_Y)
    A_w = Arena("w", tw, NB_W)
    A_t = Arena("t", ttm, NB_T)
    A_m = Arena("m", tm, NB_M)

    hT = A_h.view(0, (KC, TT), F32)
    yT = A_y.view(0, (KC, TT), BF16)
    bT = A_b.view(0, (KC, TT), BF16)

    moff = [0]

    def malloc(shape, dt, align=4):
        moff[0] = (moff[0] + align - 1) // align * align
        n = int(np.prod(shape)) * ESZ[dt]
        n = (n + 3) // 4 * 4
        t = A_m.view(moff[0], shape, dt)
        moff[0] += n
        assert moff[0] <= NB_M, moff[0]
        return t

    modT = malloc((18, KC), F32)
    gT = malloc((7, KC), F32)
    lsT = malloc((KC,), F32)
    cpk = malloc((16,), F32)
    invc = malloc((4, 16), F32)
    frq = malloc((2,), F32)
    ident4 = malloc((512,), BF16)
    onesb = malloc((128,), BF16)
    pswq = malloc((128,), F32)
    pswi = malloc((128,), F32)
    stg = malloc((2, 512), BF16)
    rp = malloc((8,), F32)
    wiT = malloc((8, 16), F32)
    kmx = malloc((8,), F32)
    smallw = malloc((1024,), F32)

    tmpA = A_t.view(0, (4, 1024), F32)
    qiT = A_t.view(0, (8, T), BF16)

    pst = [nc.alloc_psum_tensor(f"ps{i}", [P, 512], F32) for i in range(8)]

    def ps(i, n=512, p=None, dt=None):
        ap = pst[i][:, 0:n] if p is None else pst[i][p[0]:p[1], 0:n]
        return PS(ap, i)

    wslot = [0]

    def wtile(shape):
        n = int(np.prod(shape))
        assert n <= SLOT
        s = wslot[0] % NSLOT
        wslot[0] += 1
        return A_w.view(s * SLOT * 2, shape, BF16)

    def dma(eng, out_v, in_ap, kind="d"):
        sc.add(eng, lambda e, o=out_v.ap, i=in_ap: e.dma_start(out=o, in_=i), reads=(), writes=(out_v,), kind=kind)

    def dma_out(eng, out_ap, in_v):
        sc.add(eng, lambda e, o=out_ap, i=in_v.ap: e.dma_start(out=o, in_=i), reads=(in_v,), writes=(), kind="d")

    def loadw(dram_view, shape):
        t = wtile(shape)
        v = t.v()
        dma("pool", v, dram_view)
        return t

    def mm(out, lhsT, rhs, start, stop):
        sc.add("pe", lambda e, o=out.ap, l=lhsT.ap, r=rhs.ap, s0=start, s1=stop: e.matmul(o, l, r, start=s0, stop=s1),
               reads=(lhsT, rhs), writes=(out,))

    def act(out, in_, func, bias=None, scale=None, reads=()):
        kw = {}
        if bias is not None:
            kw["bias"] = bias.ap if isinstance(bias, View) else bias
        if scale is not None:
            kw["scale"] = scale.ap if isinstance(scale, View) else scale
        rd = [in_] + [x for x in (bias, scale) if isinstance(x, View)] + list(reads)
        sc.add("act", lambda e, o=out.ap, i=in_.ap, f=func, kw=kw: e.activation(o, i, f, **kw), reads=rd, writes=(out,))

    def tt(out, a, b, op, eng="dve"):
        sc.add(eng, lambda e, o=out.ap, x=a.ap, y=b.ap, op=op: e.tensor_tensor(o, x, y, op), reads=(a, b), writes=(out,))

    def ts(out, a, s1, s2, op0, op1=None, eng="dve"):
        rd = [a] + [x for x in (s1, s2) if isinstance(x, View)]
        a1 = s1.ap if isinstance(s1, View) else s1
        a2 = s2.ap if isinstance(s2, View) else s2
        if op1 is None:
            sc.add(eng, lambda e, o=out.ap, x=a.ap: e.tensor_scalar(o, x, a1, None, op0), reads=rd, writes=(out,))
        else:
            sc.add(eng, lambda e, o=out.ap, x=a.ap: e.tensor_scalar(o, x, a1, a2, op0, op1), reads=rd, writes=(out,))

    def stt(out, a, s, b, op0, op1):
        rd = [a, b] + ([s] if isinstance(s, View) else [])
        a1 = s.ap if isinstance(s, View) else s
        sc.add("dve", lambda e, o=out.ap, x=a.ap, y=b.ap: e.scalar_tensor_tensor(o, x, a1, y, op0, op1), reads=rd, writes=(out,))

    def cp(out, a, eng="dve"):
        sc.add(eng, lambda e, o=out.ap, x=a.ap: e.tensor_copy(o, x), reads=(a,), writes=(out,))

    cst = din("cst", [P, 1024])
    cstage = tmpA.v((slice(0, 1),))
    dma("sp", cstage, cst.rearrange("p (a n) -> p a n", a=1))
    cs = tmpA

    def csl(a, b):
        return cs.v((0, slice(a, b)))
    cp(ident4.v(), csl(0, 512))
    cp(onesb.v(), csl(512, 640))
    cp(pswq.v(), csl(640, 768))
    cp(pswi.v(), csl(768, 896))
    cp(frq.v(), csl(896, 898))
    cp(cpk.v(), csl(898, 914))
    cp(invc.v(), A_t.view(914 * 4, (4, 16), F32).v())
    cp(lsT.v(), csl(978, 994))
    gin = din("gT", [P, 7 * KC])
    dma("sp", gT.v(), gin.rearrange("p (a n) -> p a n", a=7))

    xT = din("xT", [D, T])
    xh = din("xhT", [D, TH])
    xv = xT.rearrange("(c p) t -> p c t", p=P)
    for c0 in range(0, KC, 4):
        dma("sp", hT.v((slice(c0, c0 + 4), slice(0, T))), xv[:, c0:c0 + 4, :])
    dma("sp", hT.v((slice(0, KC), slice(T, TT))), xh.rearrange("(c p) t -> p c t", p=P))

    cTin = din("cT", [P, KC * 4])
    awin = din("aw", [18 * D, 256])
    abin = din("ab", [P, 36])
    scf = A_t.view(8192, (KC, 4), F32)
    scb = A_t.view(8192 + 256, (KC, 4), BF16)
    abt = A_t.view(8192 + 512, (36,), F32)
    adaP = A_t.view(8192 + 1024, (36, 4), F32)
    dma("sp", scf.v(), cTin.rearrange("p (k b) -> p k b", b=4))
    dma("sp", abt.v(), abin)
    act(scb.v(), scf.v(), AF.Silu)
    pada = PS(pst[7][:, 0:144].rearrange("p (a b) -> p a b", b=4), 7)
    for lv in range(18):
        wt = loadw(awin[lv * D:(lv + 1) * D, :].rearrange("(k p) n -> p k n", p=P), (KC, 256))
        for j in range(2):
            o = PS(pst[7][:, (lv * 2 + j) * 4:(lv * 2 + j) * 4 + 4], 7)
            for k in range(KC):
                mm(o, wt.v((k, slice(j * 128, (j + 1) * 128))), scb.v((k,)), k == 0, k == KC - 1)
    sc.add("dve", lambda e: e.tensor_tensor(adaP.ap, pada.ap, abt.ap.unsqueeze(2).to_broadcast([P, 36, 4]), ALU.add),
           reads=(pada, abt.v()), writes=(adaP.v(),))
    ada_in = nc.dram_tensor("ada_in", [P, 144], F32).ap()
    ada_out = nc.dram_tensor("ada_out", [8 * P, 144], F32).ap()
    dflag = View(None, "dramflag", [(0, 1)])
    sc.add("pool", lambda e: e.dma_start(out=ada_in, in_=adaP.ap.rearrange("p a b -> p (a b)")),
           reads=(adaP.v(),), writes=(dflag,), kind="d")
    sc.add("pool", lambda e: e.collective_compute("AllGather", ALU.bypass, replica_groups=[list(range(8))],
                                                  ins=[ada_in.opt()], outs=[ada_out.opt()]),
           reads=(dflag,), writes=(dflag,), kind="cc")
    adaG = A_t.view(0, (8, 36, 4), F32)
    sc.add("pool", lambda e: e.dma_start(out=adaG.ap, in_=ada_out.rearrange("(r p) (a b) -> p r a b", p=P, b=4)),
           reads=(dflag, ), writes=(adaG.v(),), kind="d")
    adaM = A_t.view(4608, (8, 36, 4), F32)
    adaS = A_t.view(9216, (8, 36), F32)
    sel = cpk.v((slice(4, 8),))
    sc.add("dve", lambda e: e.tensor_tensor(adaM.ap, adaG.ap, sel.ap.unsqueeze(1).unsqueeze(1).to_broadcast([P, 8, 36, 4]), ALU.mult),
           reads=(adaG.v(), sel), writes=(adaM.v(),))
    sc.add("dve", lambda e: e.tensor_reduce(adaS.ap, adaM.ap, AX.X, ALU.add), reads=(adaM.v(),), writes=(adaS.v(),))
    for l in range(2):
        for s in range(3):
            base = (l * 3 + s) * 3

            def adav(k, l=l, s=s):
                a0 = (l * 9 + s * 3 + k) * 2
                return View(adaS.ap[:, :, a0:a0 + 2], A_t, adaS.v().ranges)
            gv = View(gT.ap[:, l * 3 + s, :].rearrange("p (r j) -> p r j", j=2), A_m, gT.v((l * 3 + s,)).ranges)

            def mv(i):
                return View(modT.ap[:, base + i, :].rearrange("p (r j) -> p r j", j=2), A_m, modT.v((base + i,)).ranges)
            stt(mv(0), adav(1), 1.0, gv, ALU.add, ALU.mult)
            cp(mv(1), adav(0))
            ts(mv(2), adav(2), 0.5 if s != 1 else 1.0, None, ALU.mult)

    def modv(l, s, i, c):
        return modT.v(((l * 3 + s) * 3 + i, slice(c, c + 1)))

    rstd = smallw

    def norm_mod(l, s, tchunks, final=False):
        for (t0, n) in tchunks:
            pn = ps(6, n)
            for c in range(KC):
                sq = stg.v((c % 2, slice(0, n)))
                act(sq, hT.v((c, slice(t0, t0 + n))), AF.Square)
                mm(pn, onesb.v(), sq, c == 0, c == KC - 1)
            r0 = rstd.v((slice(0, n),))
            act(r0, pn, AF.Sqrt, bias=EPS_T.v(), scale=1.0 / D)
            sc.add("dve", lambda e, o=r0.ap: e.reciprocal(o, o), reads=(r0,), writes=(r0,))
            for c in range(KC):
                hv = hT.v((c, slice(t0, t0 + n)))
                if final:
                    tmp = tmpA.v((c % 2, slice(0, n)))
                    tt(tmp, hv, r0, ALU.mult)
                    act(hv, tmp, AF.Identity, scale=gT.v((6, slice(c, c + 1))))
                else:
                    tmp = tmpA.v((c % 2, slice(0, n)))
                    tt(tmp, hv, r0, ALU.mult)
                    act(yT.v((c, slice(t0, t0 + n))), tmp, AF.Identity, bias=modv(l, s, 1, c), scale=modv(l, s, 0, c))

    EPS_T = malloc((1,), F32)
    sc.add("dve", lambda e: e.memset(EPS_T.ap, EPS), writes=(EPS_T.v(),))

    def resid_add(l, s, c, t0, n, pso):
        hv = hT.v((c, slice(t0, t0 + n)))
        stt(hv, pso, modv(l, s, 2, c), hv, ALU.mult, ALU.add)

    def ffn(l, j, tchunks):
        s = 0 if j == 0 else 2
        wgu = din("ffn_wgu", [2, 2, D, 2 * DFF])[l, j].rearrange("(k p) n -> p k n", p=P)
        wd = din("ffn_wd", [2, 2, DFF, D])[l, j]
        pi = [0]
        for (f0, nf) in FPARTS:
            for pr in range(nf // 2):
                fa = f0 + pr * 2
                wg = loadw(wgu[:, :, fa * 128:fa * 128 + 256], (KC, 256))
                wu = loadw(wgu[:, :, DFF + fa * 128:DFF + fa * 128 + 256], (KC, 256))
                for fc in range(2):
                    fl = fa + fc - f0
                    for (t0, n) in tchunks:
                        b = pi[0] % 2
                        pi[0] += 1
                        pg = ps(b * 2, n)
                        pu = ps(b * 2 + 1, n)
                        for k in range(KC):
                            mm(pg, wg.v((k, slice(fc * 128, fc * 128 + 128))), yT.v((k, slice(t0, t0 + n))), k == 0, k == KC - 1)
                        for k in range(KC):
                            mm(pu, wu.v((k, slice(fc * 128, fc * 128 + 128))), yT.v((k, slice(t0, t0 + n))), k == 0, k == KC - 1)
                        sg = tmpA.v((2 + b, slice(0, n)))
                        act(sg, pg, AF.Silu)
                        tt(bT.v((fl, slice(t0, t0 + n))), sg, pu, ALU.mult)
            for dp in range(8):
                wdt = loadw(wd[f0 * 128:(f0 + nf) * 128, dp * 256:(dp + 1) * 256].rearrange("(k p) n -> p k n", p=P), (nf, 256))
                for dc in range(2):
                    c = dp * 2 + dc
                    for (t0, n) in tchunks:
                        b = pi[0] % 2
                        pi[0] += 1
                        po = ps(4 + b, n)
                        for k in range(nf):
                            mm(po, wdt.v((k, slice(dc * 128, dc * 128 + 128))), bT.v((k, slice(t0, t0 + n))), k == 0, k == nf - 1)
                        resid_add(l, s, c, t0, n, po)

    def proj_resid(wv2d, src, l, s, tchunks):
        wv = wv2d.rearrange("(k p) n -> p k n", p=P)
        pi = 0
        for dp in range(8):
            wt = loadw(wv[:, :, dp * 256:(dp + 1) * 256], (KC, 256))
            for dc in range(2):
                c = dp * 2 + dc
                for (t0, n) in tchunks:
                    po = ps(4 + pi % 2, n)
                    pi += 1
                    for k in range(KC):
                        mm(po, wt.v((k, slice(dc * 128, dc * 128 + 128))), src.v((k, slice(t0, t0 + n))), k == 0, k == KC - 1)
                    resid_add(l, s, c, t0, n, po)

    def pool_mixer2(l):
        w_in = din("pool_w_in", [1, D, D])[0].rearrange("(k p) n -> p k n", p=P)
        w_grp = din("pool_w_grp", [1, 4, 512, 512])[0]
        w_out = din("pool_w_out", [1, D, D])[0]
        u0 = A_t.view(0, (TT,), F32)
        sA = A_t.view(TT * 4, (TT,), F32)
        sB = A_t.view(2 * TT * 4, (TT,), F32)
        pT = bT
        flag = cpk.v((slice(0, 1),))
        for dp in range(8):
            wt = loadw(w_in[:, :, dp * 256:(dp + 1) * 256], (KC, 256))
            for dc in range(2):
                c = dp * 2 + dc
                g = c // 4
                w = 2 ** (g + 1)
                for ti, (t0, n) in enumerate(WITH_HALO):
                    po = ps(ti % 2, n)
                    for k in range(KC):
                        mm(po, wt.v((k, slice(dc * 128, dc * 128 + 128))), yT.v((k, slice(t0, t0 + n))), k == 0, k == KC - 1)
                    if t0 == T:
                        act(u0.v((slice(0, TH),)), po, AF.Identity, scale=flag)
                    else:
                        act(u0.v((slice(TH + t0, TH + t0 + n),)), po, AF.Identity)
                src, step = u0, 1
                dsts = [sA, sB]
                di = 0
                while step < w:
                    lo = 2 * step
                    dst = dsts[di]
                    di = 1 - di
                    tt(dst.v((slice(lo, TT),)), src.v((slice(lo, TT),)), src.v((slice(lo - step, TT - step),)), ALU.add)
                    src, step = dst, step * 2
                stt(pT.v((c, slice(0, T))), src.v((slice(TH, TT),)), 1.0 / w, u0.v((slice(TH, TT),)), ALU.mult, ALU.subtract)
                t16 = dsts[di].v((slice(0, 16),))
                tt(t16, src.v((slice(TH, TH + 16),)), invc.v((g,)), ALU.mult)
                tt(pT.v((c, slice(0, 16))), t16, u0.v((slice(TH, TH + 16),)), ALU.subtract)
        pi = 0
        for g in range(4):
            for hp in range(2):
                wt = loadw(w_grp[g, :, hp * 256:(hp + 1) * 256].rearrange("(k p) n -> p k n", p=P), (4, 256))
                for dc in range(2):
                    c = g * 4 + hp * 2 + dc
                    for (t0, n) in MAIN:
                        po = ps(pi % 2, n)
                        pi += 1
                        for k in range(4):
                            mm(po, wt.v((k, slice(dc * 128, dc * 128 + 128))), pT.v((g * 4 + k, slice(t0, t0 + n))), k == 0, k == 3)
                        act(yT.v((c, slice(t0, t0 + n))), po, AF.Identity, scale=lsT.v((slice(c, c + 1),)))
        proj_resid(w_out, yT, l, 1, MAIN)

    def dsa_mixer(l):
        w_in = din("dsa_w_in", [1, D, 4176])[0].rearrange("(k p) n -> p k n", p=P)
        w_out = din("dsa_w_out", [1, D, D])[0]
        pos = din("pos", [P, T], I32)
        qT = bT
        RT = A_w.view(3 * SLOT * 2, (4, T), F32)
        pi32 = A_t.view(0, (T,), I32)
        pf = A_t.view(4096, (T,), F32)
        ang = A_t.view(8192, (T,), F32)
        kk = A_t.view(12288, (T,), I32)
        sc.add("sp", lambda e: e.dma_start(out=pi32.ap, in_=pos), writes=(pi32.v(),), kind="d")
        cp(pf.v(), pi32.v())
        TWO_PI = 2.0 * math.pi
        for var in range(2):
            for cs_i, shift in enumerate((math.pi / 2, 0.0)):
                dstt = RT.v((var * 2 + cs_i,))
                ts(ang.v(), pf.v(), frq.v((slice(var, var + 1),)), shift, ALU.mult, ALU.add)
                ts(kk.v(), ang.v(), 1.0 / TWO_PI, None, ALU.mult)
                cp(dstt, kk.v())
                stt(ang.v(), dstt, -TWO_PI, ang.v(), ALU.mult, ALU.add)
                ts(ang.v(), ang.v(), 3.14159, -3.14159, ALU.min, ALU.max)
                act(dstt, ang.v(), AF.Sin)
        wslot[0] = 0
        NS_SAVE = 3

        def loadw4(dram_view, shape):
            n = int(np.prod(shape))
            s_ = wslot[0] % NS_SAVE
            wslot[0] += 1
            t = A_w.view(s_ * SLOT * 2, shape, BF16)
            dma("pool", t.v(), dram_view)
            return t

        kx_in = nc.dram_tensor("kx_in", [576, T], BF16).ap()
        kx_out = nc.dram_tensor("kx_out", [2 * 576, T], BF16).ap()
        vx_in = nc.dram_tensor("vx_in", [T, 512], BF16).ap()
        vx_out = nc.dram_tensor("vx_out", [2 * T, 512], BF16).ap()
        kxf = View(None, "kxflag", [(0, 1)])
        vxf = View(None, "vxflag", [(0, 1)])

        x32b = [A_t.view(0, (512,), F32), A_t.view(2048, (512,), F32)]
        ui = [0]

        def rope_chunk(pso, npart, var, t0, dst_view):
            i = ui[0] % 2
            ui[0] += 1
            x32 = View(XR[i].ap[0:npart, :], A_m, XR[i].v().ranges)
            cp(x32, pso, eng="act" if False else "dve")
            psw = pswq if var == 0 else pswi
            pw = PS(pst[6][0:npart, 0:512], 6)
            mm(pw, View(psw.ap[0:npart, 0:npart], A_m, psw.v().ranges), x32, True, True)
            cosv = View(RT.ap[0:npart, var * 2, t0:t0 + 512], A_w, RT.v((var * 2, slice(t0, t0 + 512))).ranges)
            sinv = View(RT.ap[0:npart, var * 2 + 1, t0:t0 + 512], A_w, RT.v((var * 2 + 1, slice(t0, t0 + 512))).ranges)
            t1 = View(XS[i].ap[0:npart, :], A_m, XS[i].v().ranges)
            tt(t1, pw, sinv, ALU.mult)
            tt(x32, x32, cosv, ALU.mult)
            tt(dst_view, x32, t1, ALU.add)

        XR0 = malloc((512,), F32)
        XR = [XR0, XR0]
        XS0 = smallw.arena.view(smallw.off, (512,), F32)
        XS = [XS0, XS0]

        pj = [0]

        def nextps():
            b = pj[0] % 2
            pj[0] += 1
            return b
        for hp in range(8):
            wt = loadw4(w_in[:, :, hp * 256:(hp + 1) * 256], (KC, 256))
            for hc in range(2):
                h = hp * 2 + hc
                for (t0, n) in MAIN:
                    po = ps(nextps(), n)
                    for k in range(KC):
                        mm(po, wt.v((k, slice(hc * 128, hc * 128 + 128))), yT.v((k, slice(t0, t0 + n))), k == 0, k == KC - 1)
                    rope_chunk(po, 128, 0, t0, qT.v((h, slice(t0, t0 + n))))
        for hp in range(2):
            wt = loadw4(w_in[:, :, 2048 + hp * 256:2048 + (hp + 1) * 256], (KC, 256))
            for hc in range(2):
                g = hp * 2 + hc
                for (t0, n) in MAIN:
                    po = ps(nextps(), n)
                    for k in range(KC):
                        mm(po, wt.v((k, slice(hc * 128, hc * 128 + 128))), yT.v((k, slice(t0, t0 + n))), k == 0, k == KC - 1)
                    st = stg.v((ui[0] % 2,))
                    rope_chunk(po, 128, 0, t0, st)
                    sc.add("sp", lambda e, o=kx_in[g * 128:(g + 1) * 128, t0:t0 + n], i=st.ap: e.dma_start(out=o, in_=i),
                           reads=(st,), writes=(kxf,), kind="d")
        for hp in range(2):
            wt = loadw4(w_in[:, :, 2560 + hp * 256:2560 + (hp + 1) * 256], (KC, 256))
            for tb in range(8):
                po = ps(nextps(), 256)
                for k in range(KC):
                    mm(po, yT.v((k, slice(tb * 128, tb * 128 + 128))), wt.v((k,)), k == 0, k == KC - 1)
                st = stg.v((tb % 2, slice(0, 256)))
                cp(st, po)
                sc.add("sp", lambda e, o=vx_in[tb * 128:(tb + 1) * 128, hp * 256:(hp + 1) * 256], i=st.ap: e.dma_start(out=o, in_=i),
                       reads=(st,), writes=(vxf,), kind="d")
        wt = loadw4(w_in[:, :, 4096:4176], (KC, 80))
        for (t0, n) in MAIN:
            po = ps(nextps(), n, p=(0, 64))
            for k in range(KC):
                mm(po, wt.v((k, slice(0, 64))), yT.v((k, slice(t0, t0 + n))), k == 0, k == KC - 1)
            stt_ = stg.v((ui[0] % 2,))
            st = View(stt_.ap[0:64, :], A_m, stt_.ranges)
            rope_chunk(po, 64, 1, t0, st)
            sc.add("sp", lambda e, o=kx_in[512:576, t0:t0 + n], i=st.ap: e.dma_start(out=o, in_=i),
                   reads=(st,), writes=(kxf,), kind="d")
        for tb in range(8):
            po = ps(nextps(), 16)
            for k in range(KC):
                mm(po, yT.v((k, slice(tb * 128, tb * 128 + 128))), wt.v((k, slice(64, 80))), k == 0, k == KC - 1)
            cp(wiT.v((tb,)), po)
        sc.add("pool", lambda e: e.collective_compute("AllGather", ALU.bypass, replica_groups=[[0, 1], [2, 3], [4, 5], [6, 7]],
                                                      ins=[kx_in.opt()], outs=[kx_out.opt()]),
               reads=(kxf,), writes=(kxf,), kind="cc")
        sc.add("pool", lambda e: e.collective_compute("AllGather", ALU.bypass, replica_groups=[[0, 1], [2, 3], [4, 5], [6, 7]],
                                                      ins=[vx_in.opt()], outs=[vx_out.opt()]),
               reads=(vxf,), writes=(vxf,), kind="cc")
        for hp in range(4):
            wt = loadw4(w_in[:, :, 3072 + hp * 256:3072 + (hp + 1) * 256], (KC, 256))
            for hc in range(2):
                c = hp * 2 + hc
                for (t0, n) in MAIN:
                    po = ps(nextps(), n)
                    for k in range(KC):
                        mm(po, wt.v((k, slice(hc * 128, hc * 128 + 128))), yT.v((k, slice(t0, t0 + n))), k == 0, k == KC - 1)
                    rope_chunk(po, 128, 1, t0, qiT.v((c, slice(t0, t0 + n))))
        kTf = A_w.view(0, (4, S), BF16)
        Vf = A_w.view(16384, (16, 512), BF16)
        ki2 = A_w.view(32768, (S,), BF16)
        mb = A_w.view(36864, (S,), BF16)
        ptb = [A_w.view(40960 + i * 1024, (512,), BF16) for i in range(3)]
        Rr = [A_y.view(28672 + i * 2048, (512,), F32) for i in range(2)]
        kxo = kx_out.rearrange("(r x) s -> x r s", r=2)
        for g in range(4):
            sc.add("sp", lambda e, o=kTf.ap[:, g, :].rearrange("p (r s) -> p r s", r=2), i=kxo[g * 128:(g + 1) * 128]: e.dma_start(out=o, in_=i),
                   reads=(kxf,), writes=(kTf.v((g,)),), kind="d")
        for hh in range(2):
            kv = View(ki2.ap[hh * 64:(hh + 1) * 64, :].rearrange("p (r s) -> p r s", r=2), A_w, ki2.v().ranges)
            sc.add("sp", lambda e, o=kv.ap, i=kxo[512:576]: e.dma_start(out=o, in_=i), reads=(kxf,), writes=(kv,), kind="d")
        sc.add("sp", lambda e: e.dma_start(out=Vf.ap, in_=vx_out.rearrange("(b p) f -> p b f", p=P)), reads=(vxf,), writes=(Vf.v(),), kind="d")
        accs = [A_y.view(0, (S,), F32), A_y.view(8192, (S,), F32)]
        Wk = A_y.view(16384, (S,), F32)
        diag = A_y.view(24576, (16, 128), BF16)
        mbs = [mb, A_y.view(28672, (S,), BF16)]
        ksq = A_m.view(smallw.off, (512,), BF16)
        tmpR = A_m.view(XR0.off, (512,), F32)
        Rr = [A_m.view(stg.off, (512,), BF16), A_m.view(stg.off + 1024, (512,), BF16)]
        mt = malloc((128,), F32, align=512)
        m8 = A_m.view(mt.off, (8,), F32)
        thr = A_m.view(mt.off + 32, (2,), F32)
        negb = malloc((512,), BF16, align=512)
        tri32 = A_y.view(16384, (128,), F32)
        zer32 = A_y.view(16384 + 512, (128,), F32)
        T0 = malloc((128,), BF16)
        T1 = malloc((128,), BF16)
        negrow = malloc((128,), BF16)
        omh = malloc((2,), F32)
        sc.add("dve", lambda e: e.memset(zer32.ap, 0.0), writes=(zer32.v(),))
        sc.add("pool", lambda e: e.affine_select(tri32.ap, zer32.ap, [[-1, 128]], ALU.is_ge, NEG, base=0, channel_multiplier=1),
               reads=(zer32.v(),), writes=(tri32.v(),))
        hfv = cpk.v((slice(0, 1),))
        ts(omh.v((slice(0, 1),)), hfv, -1.0, 1.0, ALU.mult, ALU.add)
        ts(omh.v((slice(1, 2),)), omh.v((slice(0, 1),)), NEG, None, ALU.mult)
        ts(T0.v(), tri32.v(), omh.v((slice(0, 1),)), None, ALU.mult)
        ts(T1.v(), tri32.v(), hfv, omh.v((slice(1, 2),)), ALU.mult, ALU.add)
        nrv = View(negrow.ap[0:1, :], A_m, negrow.v().ranges)
        sc.add("dve", lambda e: e.memset(nrv.ap, 1.0), writes=(nrv,))
        ts(nrv, nrv, View(omh.ap[0:1, 1:2], A_m, omh.v().ranges), None, ALU.mult)
        for i in range(8):
            ts(rp.v((slice(i, i + 1),)), cpk.v((slice(1, 2),)), float(i * 128), None, ALU.add)
        for g in range(4):
            for sb4 in range(4):
                kq = ksq.v()
                act(kq, kTf.v((g, slice(sb4 * 512, sb4 * 512 + 512))), AF.Square)
                pk = PS(pst[6][0:1, 0:512], 6)
                mm(pk, View(onesb.ap[:, 0:1], A_m, onesb.v().ranges), kq, True, True)
                dstk = View(smallw.ap[0:1, 256 + g * 4 + sb4:256 + g * 4 + sb4 + 1], A_m, smallw.v((slice(256, 512),)).ranges)
                sc.add("dve", lambda e, o=dstk.ap, i=pk.ap: e.tensor_reduce(o, i, AX.X, ALU.max), reads=(pk,), writes=(dstk,))
            src4 = View(smallw.ap[0:1, 256 + g * 4:256 + g * 4 + 4], A_m, smallw.v((slice(256, 512),)).ranges)
            kd = View(kmx.ap[0:1, g:g + 1], A_m, kmx.v().ranges)
            sc.add("dve", lambda e, o=kd.ap, i=src4.ap: e.tensor_reduce(o, i, AX.X, ALU.max), reads=(src4,), writes=(kd,))
        km4 = View(kmx.ap[0:1, 0:4], A_m, kmx.v().ranges)
        km4s = View(kmx.ap[0:1, 4:8], A_m, kmx.v().ranges)
        act(km4s, km4, AF.Sqrt)
        ts(km4s, km4s, -1.03, None, ALU.mult)
        SCALE = 1.0 / math.sqrt(128.0)
        def stageA(i):
            nkb = 9 + i
            nk = nkb * 128
            acc = accs[i % 2]
            sc.add("dve", lambda e, o=diag.ap, a_=ident4.ap[:, 0:128].unsqueeze(1).to_broadcast([P, 16, 128]),
                   b_=wiT.ap[:, i, :].unsqueeze(2).to_broadcast([P, 16, 128]): e.tensor_tensor(o, a_, b_, ALU.mult),
                   reads=(ident4.v(), wiT.v((i,))), writes=(diag.v(),))
            ri = 0
            k0 = 0
            while k0 < nk:
                n = min(512, nk - k0)
                pacc = ps(7, n)
                bias_ops = []
                for sb in range(k0 // 128, (k0 + n) // 128):
                    c0 = sb * 128 - k0
                    osub = PS(pst[7][:, c0:c0 + 128], 7)
                    if sb == i:
                        bias_ops.append((osub, ident4.v((slice(0, 128),)), T0.v()))
                    elif sb == 8 + i:
                        bias_ops.append((osub, ident4.v((slice(0, 128),)), T1.v()))
                    elif i < sb < 8 + i:
                        bias_ops.append((osub, View(onesb.ap[0:1, :], A_m, onesb.v().ranges), nrv))
                for h in range(16):
                    c, pb = h // 2, (h % 2) * 64
                    b = nextps()
                    px = ps(b, n)
                    lh = View(qiT.ap[pb:pb + 64, c, i * 128:(i + 1) * 128], A_t, qiT.v((c, slice(i * 128, (i + 1) * 128))).ranges)
                    rh = View(ki2.ap[pb:pb + 64, k0:k0 + n], A_w, ki2.v((slice(k0, k0 + n),)).ranges)
                    mm(px, lh, rh, True, True)
                    rr = Rr[ri % 2].v((slice(0, n),))
                    ri += 1
                    act(rr, px, AF.Relu)
                    mm(pacc, diag.v((h,)), rr, h == 0, h == 15 and not bias_ops)
                for bi_, (osub, lt, rt) in enumerate(bias_ops):
                    mm(osub, lt, rt, False, bi_ == len(bias_ops) - 1)
                act(acc.v((slice(k0, k0 + n),)), pacc, AF.Identity)
                k0 += n

        def stageB(i):
            nkb = 9 + i
            nk = nkb * 128
            acc = accs[i % 2]
            mbi = mbs[i % 2]
            for r in range(32):
                srcv = acc.v((slice(0, nk),)) if r == 0 else Wk.v((slice(0, nk),))
                sc.add("dve", lambda e, o=m8.ap, s_=srcv.ap: e.max(out=o, in_=s_), reads=(srcv,), writes=(m8.v(),))
                if r < 31:
                    wv = Wk.v((slice(0, nk),))
                    sc.add("dve", lambda e, o=wv.ap, s_=srcv.ap, m=m8.ap: e.match_replace(out=o, in_to_replace=m, in_values=s_, imm_value=-3.0e38),
                           reads=(srcv, m8.v()), writes=(wv,))
            ts(thr.v((slice(0, 1),)), m8.v((slice(7, 8),)), -1.0e29, None, ALU.max)
            ts(mbi.v((slice(0, nk),)), acc.v((slice(0, nk),)), thr.v((slice(0, 1),)), NEGB, ALU.is_lt, ALU.mult)

        def stageC(i):
            nkb = 9 + i
            mbi = mbs[i % 2]
            for g in range(4):
                qblk = View(qT.ap[:, 4 * g:4 * g + 4, i * 128:(i + 1) * 128], A_b,
                            qT.v((slice(4 * g, 4 * g + 4), slice(i * 128, (i + 1) * 128))).ranges)
                sq4 = View(ksq.ap.rearrange("p (a b) -> p a b", a=4), A_m, ksq.v().ranges)
                act(sq4, qblk, AF.Square)
                pq = PS(pst[6][0:1, 0:512], 6)
                mm(pq, View(onesb.ap[:, 0:1], A_m, onesb.v().ranges), ksq.v(), True, True)
                nb0 = View(smallw.ap[0:1, 512:1024], A_m, smallw.v((slice(512, 1024),)).ranges)
                act(nb0, pq, AF.Sqrt)
                nbv = View(negb.ap[0:1, :], A_m, negb.v().ranges)
                ts(nbv, nb0, View(kmx.ap[0:1, 4 + g:5 + g], A_m, kmx.v().ranges), None, ALU.mult)
                po = ps(4)
                pd = ps(5)
                for sb in range(nkb):
                    pS = ps(2 + sb % 2)
                    mm(pS, kTf.v((g, slice(sb * 128, sb * 128 + 128))), qblk, True, False)
                    mm(pS, mbi.v((slice(sb * 128, sb * 128 + 128),)), ident4.v(), False, False)
                    mm(pS, View(onesb.ap[0:1, :], A_m, onesb.v().ranges), nbv, False, True)
                    pt = ptb[sb % 3].v()
                    act(pt, pS, AF.Exp, scale=SCALE)
                    mm(po, Vf.v((sb, slice(g * 128, g * 128 + 128))), pt, sb == 0, sb == nkb - 1)
                    mm(pd, onesb.v(), pt, sb == 0, sb == nkb - 1)
                rd = tmpR.v()
                sc.add("dve", lambda e, o=rd.ap, i_=pd.ap: e.reciprocal(o, i_), reads=(pd,), writes=(rd,))
                ov = View(qblk.ap, A_b, qblk.ranges)
                sc.add("dve", lambda e, o=ov.ap, a=po.ap.rearrange("p (a b) -> p a b", a=4), b_=rd.ap.rearrange("p (a b) -> p a b", a=4):
                       e.tensor_tensor(o, a, b_, ALU.mult), reads=(po, rd), writes=(ov,))

        def record(fn, *args):
            ops_ = []
            orig = sc.add
            sc.add = lambda *a, **k: ops_.append((a, k))
            try:
                fn(*args)
            finally:
                sc.add = orig
            return ops_

        def merge_emit(streams):
            streams = [s_ for s_ in streams if s_]
            total = max(len(s_) for s_ in streams)
            idx = [0] * len(streams)
            for step in range(total):
                for si_, s_ in enumerate(streams):
                    target = (step + 1) * len(s_) // total
                    while idx[si_] < target:
                        a_, k_ = s_[idx[si_]]
                        sc.add(*a_, **k_)
                        idx[si_] += 1

        stageA(0)
        for st_ in range(8):
            streams = [record(stageB, st_)]
            if st_ + 1 < 8:
                streams.append(record(stageA, st_ + 1))
            if st_ >= 1:
                streams.append(record(stageC, st_ - 1))
            merge_emit(streams)
        stageC(7)
        wslot[0] = 0
        proj_resid(w_out, qT, l, 1, MAIN)


    stages = ["ffn1_0", "pool", "ffn2_0", "ffn1_1", "dsa", "full"]
    if stop == "pool_only":
        norm_mod(0, 1, WITH_HALO)
        pool_mixer2(0)
    elif stop == "dsa_only":
        norm_mod(1, 1, MAIN)
        dsa_mixer(1)
    else:
        si = stages.index(stop)
        norm_mod(0, 0, HALO3)
        ffn(0, 0, HALO3)
        if si >= 1:
            norm_mod(0, 1, HALO3)
            pool_mixer2(0)
        if si >= 2:
            norm_mod(0, 2, MAIN)
            ffn(0, 1, MAIN)
        if si >= 3:
            norm_mod(1, 0, MAIN)
            ffn(1, 0, MAIN)
        if si >= 4:
            norm_mod(1, 1, MAIN)
            dsa_mixer(1)
        if si >= 5:
            norm_mod(1, 2, MAIN)
            ffn(1, 1, MAIN)
            norm_mod(0, 0, MAIN, final=True)
    ov = outT.rearrange("(c p) t -> p c t", p=P)
    for c0 in range(0, KC, 4):
        dma_out("sp", ov[:, c0:c0 + 4, :], hT.v((slice(c0, c0 + 4), slice(0, T))))
    last = [op for op in sc.q["sp"] if op["kind"] == "d"][-4:]
    sc.finalize()
    sems = {}
    for e in Sched.COMPUTE:
        sems[("c", e)] = nc.alloc_semaphore(f"c_{e}")
    for e in ("sp", "pool"):
        for i in range(sc.nring):
            sems[("d", e, i)] = nc.alloc_semaphore(f"d_{e}_{i}")
    sems[("cc",)] = nc.alloc_semaphore("ccs")
    with nc.Block() as block:
        def final_wait(e):
            for op in last:
                e.wait_ge(sems[op["sem"]], op["val"])
            return e.nop()
        fop = dict(id=-1, eng="sp", fn=final_wait, kind="c", deps=set(), need=[], signal=False, idx=len(sc.q["sp"]))
        sc.q["sp"].append(fop)
        sc.emit(nc, block, sems)
    return nc, list(dram.keys())


def _consts(hf):
    cst = np.zeros((P, 1024), np.float32)
    eye = np.eye(P, dtype=np.float32)
    cst[:, 0:512] = np.tile(eye, (1, 4))
    cst[:, 512:640] = 1.0
    def psw(block, half):
        m = np.zeros((P, P), np.float32)
        for b0 in range(0, P, block):
            for dd in range(half):
                m[b0 + dd + half, b0 + dd] = -1.0
                m[b0 + dd, b0 + dd + half] = 1.0
        return m
    cst[:, 640:768] = psw(128, 16)
    cst[:, 768:896] = psw(64, 8)
    fq = np.zeros(P, np.float32)
    fq[0:16] = THETA ** (-np.arange(16, dtype=np.float32) / 16)
    fq[16:32] = fq[0:16]
    fi = np.zeros(P, np.float32)
    for b0 in (0, 64):
        fi[b0:b0 + 8] = THETA ** (-np.arange(8, dtype=np.float32) / 8)
        fi[b0 + 8:b0 + 16] = fi[b0:b0 + 8]
    cst[:, 896] = fq
    cst[:, 897] = fi
    return cst


def kernel(x, c, positions, ada_w, ada_b, norm_g, final_g, ffn_wgu, ffn_wd,
           pool_w_in, pool_w_grp, pool_scale, pool_w_out, dsa_w_in, dsa_w_out, _stop="full"):
    nc, names = build(_stop)
    x = np.asarray(x, np.float32)
    f = lambda a: np.ascontiguousarray(np.asarray(a))
    gT = np.concatenate([np.asarray(norm_g, np.float32).reshape(6, D), np.asarray(final_g, np.float32).reshape(1, D)], 0)
    gT = f(gT.reshape(7, KC, P).transpose(2, 0, 1).reshape(P, 7 * KC))
    cT = f(np.asarray(c, np.float32).reshape(4, KC, P).transpose(2, 1, 0).reshape(P, KC * 4))
    aw = np.asarray(ada_w, np.float32).reshape(2, D, 9, D)
    ab = np.asarray(ada_b, np.float32).reshape(2, 9, D)
    ls = np.asarray(pool_scale, np.float32).reshape(KC, P).T
    in_maps = []
    for core in range(8):
        b, hf = core // 2, core % 2
        m = {}
        m["xT"] = f(x[b, hf * T:(hf + 1) * T, :].T)
        m["xhT"] = f(x[b, T - TH:T, :].T) if hf == 1 else np.zeros((D, TH), np.float32)
        cst = _consts(hf)
        cst[:, 898] = float(hf)
        cst[:, 899] = np.arange(P, dtype=np.float32) + hf * T
        cst[:, 902 + b] = 1.0
        for g, w in enumerate((2, 4, 8, 16)):
            tpos = np.arange(16) + hf * T
            cst[:, 914 + g * 16:914 + (g + 1) * 16] = (1.0 / np.minimum(tpos + 1, w)).astype(np.float32)[None, :]
        cst[:, 978:994] = ls
        m["cst"] = cst
        m["gT"] = gT
        m["cT"] = cT
        m["aw"] = f(aw[:, :, :, core * 256:(core + 1) * 256].transpose(0, 2, 1, 3).reshape(18 * D, 256))
        m["ab"] = f(ab[:, :, core * 256:(core + 1) * 256].reshape(18, 2, P).transpose(2, 0, 1).reshape(P, 36))
        m["pos"] = f(np.tile(np.asarray(positions, np.int32)[b, hf * T:(hf + 1) * T].reshape(1, T), (P, 1)))
        m["ffn_wgu"] = f(ffn_wgu)
        m["ffn_wd"] = f(ffn_wd)
        m["pool_w_in"] = f(pool_w_in)
        m["pool_w_grp"] = f(pool_w_grp)
        m["pool_w_out"] = f(pool_w_out)
        m["dsa_w_in"] = f(dsa_w_in)
        m["dsa_w_out"] = f(dsa_w_out)
        in_maps.append({k: m[k] for k in names})
    res = run_bass_kernel_spmd(nc, in_maps, core_ids=list(range(8)))
    out = np.zeros((4, S, D), np.float32)
    for core in range(8):
        b, hf = core // 2, core % 2
        out[b, hf * T:(hf + 1) * T, :] = res.results[core]["outT"].T
    return out
```
